# Optimizing a Trainium2 kernel written in Bass

```python
import math
import jax, jax.numpy as jnp
from jax import lax
import numpy as np

D_MODEL = 2048
BATCH = 2
SEQ = 4096
DEPTH = 4

PLE_DIM = 256

RWKV_HEADS = 12
RWKV_HEAD_DIM = 64
RWKV_DIM = RWKV_HEADS * RWKV_HEAD_DIM
RWKV_DECAY_LORA = 64
RWKV_AAA_LORA = 64
RWKV_GATE_LORA = 128
RWKV_GN_EPS = 64e-5
RWKV_IN_SIZES = (RWKV_DIM, RWKV_DIM, RWKV_DIM, RWKV_DECAY_LORA, RWKV_AAA_LORA, RWKV_GATE_LORA)
RWKV_IN_DIM = sum(RWKV_IN_SIZES)

DIL_PATTERNS = ((128, 1), (512, 4), (2048, 16))
DIL_N_GROUPS = 3
DIL_HEADS_PER_GROUP = 4
DIL_HEADS = DIL_N_GROUPS * DIL_HEADS_PER_GROUP
DIL_HEAD_DIM = 128
DIL_QKV_DIM = DIL_HEADS * DIL_HEAD_DIM
DIL_OUT_DIM = DIL_HEADS_PER_GROUP * DIL_HEAD_DIM
DIL_BLOCK = 128
ALIBI_MAX_BIAS = 8.0

MLA_HEADS = 6
MLA_NOPE_DIM = 128
MLA_ROPE_DIM = 64
MLA_V_DIM = 128
MLA_Q_LORA = 512
MLA_KV_LORA = 256
MLA_OUT_DIM = MLA_HEADS * MLA_V_DIM
ROPE_THETA = 10000.0
Q_BLOCK = 128

MIX_DIM = RWKV_DIM + DIL_OUT_DIM + MLA_OUT_DIM
IN_SIZES = (RWKV_IN_DIM, DIL_QKV_DIM, DIL_QKV_DIM, DIL_QKV_DIM, MLA_Q_LORA, MLA_KV_LORA, MLA_ROPE_DIM)
IN_DIM = sum(IN_SIZES)

N_EXPERTS = 32
N_EXPERT_GROUPS = 8
EXPERTS_PER_GROUP = 4
TOP_K = 2
D_EXPERT = 512
MOE_BLOCK = 128

DEEPNORM_ALPHA = (2 * DEPTH) ** 0.25
DEEPNORM_BETA = (8 * DEPTH) ** -0.25
LN_EPS = 1e-5
RMS_EPS = 1e-6
NEG_INF = -1e30

kernel_name = "hybrid_rwkv7_dilated_mla_grouped_moe_deepnorm"


def _split(z, sizes):
    return jnp.split(z, np.cumsum(sizes)[:-1].tolist(), axis=-1)


def _layernorm(x, g, b):
    xf = x.astype(jnp.float32)
    mu = xf.mean(-1, keepdims=True)
    var = jnp.square(xf - mu).mean(-1, keepdims=True)
    return ((xf - mu) * lax.rsqrt(var + LN_EPS) * g + b).astype(x.dtype)


def _rmsnorm(x, g):
    xf = x.astype(jnp.float32)
    return (xf * lax.rsqrt(jnp.mean(xf * xf, -1, keepdims=True) + RMS_EPS) * g).astype(x.dtype)


def _rope(t, pos):
    half = MLA_ROPE_DIM // 2
    inv = ROPE_THETA ** (-jnp.arange(half, dtype=jnp.float32) / half)
    ang = pos.astype(jnp.float32)[:, None] * inv[None, :]
    cos, sin = jnp.cos(ang)[None, :, None, :], jnp.sin(ang)[None, :, None, :]
    t1, t2 = t[..., :half].astype(jnp.float32), t[..., half:].astype(jnp.float32)
    return jnp.concatenate([t1 * cos - t2 * sin, t1 * sin + t2 * cos], axis=-1)


def _rwkv7(z, mu, w0, w_up, a0, a_up, g_up, k_k, k_a, r_k, gn_g, gn_b):
    B, S, _ = z.shape
    H, E = RWKV_HEADS, RWKV_HEAD_DIM
    z_prev = jnp.pad(z[:, :-1], ((0, 0), (1, 0), (0, 0)))
    z = z + (z_prev - z) * mu
    r, k, v, wd, ad, gd = _split(z, RWKV_IN_SIZES)
    w = -jax.nn.softplus(-(w0 + jnp.tanh(wd) @ w_up)) - 0.5
    decay = jnp.exp(-jnp.exp(w.astype(jnp.float32)))
    a = jax.nn.sigmoid(a0 + ad @ a_up)
    g = jax.nn.sigmoid(gd) @ g_up
    heads = lambda t: t.reshape(B, S, H, E).astype(jnp.float32)
    kk = heads(k * k_k)
    kk = kk / jnp.maximum(jnp.sqrt(jnp.sum(kk * kk, -1, keepdims=True)), 1e-12)
    k = k * (1 + (a - 1) * k_a)
    rh, kh, vh, ah, wh = heads(r), heads(k), heads(v), heads(a), heads(decay)

    def step(state, inp):
        r_t, w_t, k_t, v_t, a_t, b_t = inp
        sa = jnp.einsum('bhij,bhj->bhi', state, a_t)
        state = (state * w_t[:, :, None, :] + sa[..., None] * b_t[:, :, None, :]
                 + v_t[..., None] * k_t[:, :, None, :])
        return state, jnp.einsum('bhij,bhj->bhi', state, r_t)

    tm = lambda t: jnp.moveaxis(t, 1, 0)
    s0 = jnp.zeros((B, H, E, E), jnp.float32)
    _, y = lax.scan(step, s0, (tm(rh), tm(wh), tm(kh), tm(vh), tm(-kk), tm(kk * ah)))
    y = jnp.moveaxis(y, 0, 1)
    ym = y.mean(-1, keepdims=True)
    yv = jnp.square(y - ym).mean(-1, keepdims=True)
    y = ((y - ym) * lax.rsqrt(yv + RWKV_GN_EPS)).reshape(B, S, RWKV_DIM) * gn_g + gn_b
    bonus = jnp.sum(rh * kh * r_k, -1, keepdims=True) * vh
    y = y + bonus.reshape(B, S, RWKV_DIM)
    return (y * g).astype(z.dtype)


def _dilated_group(q, k, v, window, dilation, slopes):
    B, S, Hg, E = q.shape
    n_neigh = window // dilation
    L = S // dilation
    nb = -(-L // DIL_BLOCK)
    Lp = nb * DIL_BLOCK

    def sub(t):
        t = t.reshape(B, L, dilation, Hg, E).transpose(0, 2, 1, 3, 4)
        t = jnp.pad(t, ((0, 0), (0, 0), (0, Lp - L), (0, 0), (0, 0)))
        return t.reshape(B, dilation, nb, DIL_BLOCK, Hg, E)

    def band(t):
        prev = jnp.pad(t, ((0, 0), (0, 0), (1, 0), (0, 0), (0, 0), (0, 0)))[:, :, :-1]
        return jnp.concatenate([prev, t], axis=3)

    qb, kw, vw = sub(q), band(sub(k)), band(sub(v))
    s = jnp.einsum('bdnqhe,bdnkhe->bdnhqk', qb, kw) * (E ** -0.5)
    qi = jnp.arange(DIL_BLOCK)
    ki = jnp.arange(2 * DIL_BLOCK)
    rel = qi[:, None] + DIL_BLOCK - ki[None, :]
    key_idx = jnp.arange(nb)[:, None] * DIL_BLOCK - DIL_BLOCK + ki[None, :]
    valid = (rel >= 0)[None] & (rel <= n_neigh)[None] & (key_idx >= 0)[:, None, :]
    dist = (rel * dilation).astype(jnp.float32)
    s = s - slopes[:, None, None] * dist[None]
    s = jnp.where(valid[:, None], s, NEG_INF)
    m = s.max(-1, keepdims=True)
    e = jnp.exp(s - m)
    den = e.sum(-1, keepdims=True)
    o = jnp.einsum('bdnhqk,bdnkhe->bdnqhe', e / den, vw)
    lse = (m + jnp.log(den))[..., 0]

    def unsub(t):
        t = t[:, :, :L]
        return jnp.swapaxes(t, 1, 2).reshape((B, S) + t.shape[3:])

    o = unsub(o.reshape(B, dilation, Lp, Hg, E))
    lse = unsub(jnp.swapaxes(lse, 3, 4).reshape(B, dilation, Lp, Hg))
    return o, lse


def _dilated_attention(zq, zk, zv):
    B, S, _ = zq.shape
    shp = (B, S, DIL_N_GROUPS, DIL_HEADS_PER_GROUP, DIL_HEAD_DIM)
    q = zq.reshape(shp).astype(jnp.float32)
    k = zk.reshape(shp).astype(jnp.float32)
    v = zv.reshape(shp).astype(jnp.float32)
    slopes = jnp.exp2(-ALIBI_MAX_BIAS * jnp.arange(1, DIL_HEADS + 1, dtype=jnp.float32) / DIL_HEADS)
    slopes = slopes.reshape(DIL_N_GROUPS, DIL_HEADS_PER_GROUP)
    outs, lses = [], []
    for gi, (window, dilation) in enumerate(DIL_PATTERNS):
        o, l = _dilated_group(q[:, :, gi], k[:, :, gi], v[:, :, gi], window, dilation, slopes[gi])
        outs.append(o)
        lses.append(l)
    wts = jax.nn.softmax(jnp.stack(lses), axis=0)
    out = jnp.sum(wts[..., None] * jnp.stack(outs), axis=0)
    return out.reshape(B, S, DIL_OUT_DIM).astype(zq.dtype)


def _mla(zq, zkv, zkr, qa_g, w_uq, kva_g, w_ukv):
    B, S, _ = zq.shape
    H, QD = MLA_HEADS, MLA_NOPE_DIM + MLA_ROPE_DIM
    pos = jnp.arange(S)
    q = (_rmsnorm(zq, qa_g) @ w_uq).reshape(B, S, H, QD)
    kv = (_rmsnorm(zkv, kva_g) @ w_ukv).reshape(B, S, H, MLA_NOPE_DIM + MLA_V_DIM)
    k_nope, v = kv[..., :MLA_NOPE_DIM], kv[..., MLA_NOPE_DIM:]
    k_rope = _rope(zkr[:, :, None, :], pos)
    qf = jnp.concatenate([q[..., :MLA_NOPE_DIM].astype(jnp.float32), _rope(q[..., MLA_NOPE_DIM:], pos)], -1)
    kf = jnp.concatenate([k_nope.astype(jnp.float32), jnp.broadcast_to(k_rope, (B, S, H, MLA_ROPE_DIM))], -1)
    vf = v.astype(jnp.float32)
    scale = QD ** -0.5
    nq = S // Q_BLOCK
    qb = jnp.moveaxis(qf.reshape(B, nq, Q_BLOCK, H, QD), 1, 0)

    def block(args):
        q_blk, bi = args
        s = jnp.einsum('bqhe,bkhe->bhqk', q_blk, kf) * scale
        qpos = bi * Q_BLOCK + jnp.arange(Q_BLOCK)
        s = jnp.where(pos[None, :] <= qpos[:, None], s, NEG_INF)
        return jnp.einsum('bhqk,bkhe->bqhe', jax.nn.softmax(s, axis=-1), vf)

    o = lax.map(block, (qb, jnp.arange(nq)))
    return jnp.moveaxis(o, 0, 1).reshape(B, S, MLA_OUT_DIM).astype(zq.dtype)


def _moe(x, router_w, router_b, w_gate, w_up, w_down):
    B, S, D = x.shape
    N = B * S
    xt = x.reshape(N, D)
    scores = jax.nn.sigmoid((xt @ router_w).astype(jnp.float32))
    sel = scores + router_b.astype(jnp.float32)
    grp_score = lax.top_k(sel.reshape(N, N_EXPERT_GROUPS, EXPERTS_PER_GROUP), TOP_K)[0].sum(-1)
    g_best = jnp.argmax(grp_score, axis=-1)
    in_group = (jnp.arange(N_EXPERTS) // EXPERTS_PER_GROUP)[None, :] == g_best[:, None]
    _, idx = lax.top_k(jnp.where(in_group, sel, -jnp.inf), TOP_K)
    gate = jnp.take_along_axis(scores, idx, axis=-1)
    gate = gate / gate.sum(-1, keepdims=True)

    A = N * TOP_K
    e_flat = idx.reshape(A)
    tok = jnp.repeat(jnp.arange(N, dtype=jnp.int32), TOP_K)
    order = jnp.argsort(e_flat)
    e_s, tok_s, gate_s = e_flat[order], tok[order], gate.reshape(A)[order]
    counts = jnp.zeros((N_EXPERTS,), jnp.int32).at[e_flat].add(1)
    padded = (counts + MOE_BLOCK - 1) // MOE_BLOCK * MOE_BLOCK
    pad_end = jnp.cumsum(padded)
    pad_start = pad_end - padded
    start = jnp.cumsum(counts) - counts
    dest = pad_start[e_s] + jnp.arange(A, dtype=jnp.int32) - start[e_s]
    nb = (A + N_EXPERTS * (MOE_BLOCK - 1) + MOE_BLOCK - 1) // MOE_BLOCK
    P = nb * MOE_BLOCK
    buf_tok = jnp.full((P,), N, jnp.int32).at[dest].set(tok_s)
    xb = jnp.concatenate([xt, jnp.zeros((1, D), xt.dtype)], axis=0)[buf_tok].reshape(nb, MOE_BLOCK, D)
    blk_e = jnp.minimum(jnp.searchsorted(pad_end, jnp.arange(nb, dtype=jnp.int32) * MOE_BLOCK, side='right'),
                        N_EXPERTS - 1)

    def expert_block(args):
        xblk, e = args
        h = jax.nn.silu(xblk @ w_gate[e]) * (xblk @ w_up[e])
        return h @ w_down[e]

    yb = lax.map(expert_block, (xb, blk_e)).reshape(P, D)
    y = jax.ops.segment_sum(yb[dest] * gate_s[:, None].astype(yb.dtype), tok_s, num_segments=N)
    return y.reshape(B, S, D).astype(x.dtype)


def setup_inputs(seed: int = 0) -> dict:
    key = jax.random.key(seed)
    ks = iter(jax.random.split(key, 40))
    L, D = DEPTH, D_MODEL

    def nrm(shape, scale):
        return jax.random.normal(next(ks), shape, jnp.float32) * scale

    def uni(shape, lo, hi):
        return jax.random.uniform(next(ks), shape, jnp.float32, lo, hi)

    return {
        "x": nrm((BATCH, SEQ, D), 1.0),
        "p": nrm((DEPTH, BATCH, SEQ, PLE_DIM), 1.0),
        "w_in": nrm((L, D, IN_DIM), D ** -0.5),
        "rwkv_mu": uni((L, RWKV_IN_DIM), 0.0, 1.0),
        "rwkv_w0": uni((L, RWKV_DIM), -6.0, 1.0),
        "rwkv_w_up": nrm((L, RWKV_DECAY_LORA, RWKV_DIM), 0.1 * RWKV_DECAY_LORA ** -0.5),
        "rwkv_a0": nrm((L, RWKV_DIM), 0.1),
        "rwkv_a_up": nrm((L, RWKV_AAA_LORA, RWKV_DIM), 0.5 * RWKV_AAA_LORA ** -0.5),
        "rwkv_g_up": nrm((L, RWKV_GATE_LORA, RWKV_DIM), RWKV_GATE_LORA ** -0.5),
        "rwkv_k_k": 0.85 + nrm((L, RWKV_DIM), 0.05),
        "rwkv_k_a": 1.0 + nrm((L, RWKV_DIM), 0.05),
        "rwkv_r_k": nrm((L, RWKV_HEADS, RWKV_HEAD_DIM), 0.1),
        "rwkv_gn_g": 1.0 + nrm((L, RWKV_DIM), 0.05),
        "rwkv_gn_b": nrm((L, RWKV_DIM), 0.02),
        "mla_qa_g": 1.0 + nrm((L, MLA_Q_LORA), 0.05),
        "mla_w_uq": nrm((L, MLA_Q_LORA, MLA_HEADS * (MLA_NOPE_DIM + MLA_ROPE_DIM)), MLA_Q_LORA ** -0.5),
        "mla_kva_g": 1.0 + nrm((L, MLA_KV_LORA), 0.05),
        "mla_w_ukv": nrm((L, MLA_KV_LORA, MLA_HEADS * (MLA_NOPE_DIM + MLA_V_DIM)), MLA_KV_LORA ** -0.5),
        "w_out": nrm((L, MIX_DIM, D), DEEPNORM_BETA * MIX_DIM ** -0.5),
        "ln1_g": 1.0 + nrm((L, D), 0.05),
        "ln1_b": nrm((L, D), 0.02),
        "router_w": nrm((D, N_EXPERTS), D ** -0.5),
        "router_b": nrm((N_EXPERTS,), 0.01),
        "moe_w_gate": nrm((L, N_EXPERTS, D, D_EXPERT), D ** -0.5),
        "moe_w_up": nrm((L, N_EXPERTS, D, D_EXPERT), D ** -0.5),
        "moe_w_down": nrm((L, N_EXPERTS, D_EXPERT, D), DEEPNORM_BETA * D_EXPERT ** -0.5),
        "ln2_g": 1.0 + nrm((L, D), 0.05),
        "ln2_b": nrm((L, D), 0.02),
        "ple_w_proj": nrm((L, PLE_DIM, D), PLE_DIM ** -0.5),
        "ple_w_gate": nrm((L, D, D), D ** -0.5),
    }


def reference(x, p, w_in, rwkv_mu, rwkv_w0, rwkv_w_up, rwkv_a0, rwkv_a_up, rwkv_g_up,
              rwkv_k_k, rwkv_k_a, rwkv_r_k, rwkv_gn_g, rwkv_gn_b,
              mla_qa_g, mla_w_uq, mla_kva_g, mla_w_ukv, w_out, ln1_g, ln1_b,
              router_w, router_b, moe_w_gate, moe_w_up, moe_w_down, ln2_g, ln2_b,
              ple_w_proj, ple_w_gate):
    for i in range(DEPTH):
        z = x @ w_in[i]
        z_rwkv, z_dq, z_dk, z_dv, z_qa, z_kva, z_kr = _split(z, IN_SIZES)
        y_a = _rwkv7(z_rwkv, rwkv_mu[i], rwkv_w0[i], rwkv_w_up[i], rwkv_a0[i], rwkv_a_up[i],
                     rwkv_g_up[i], rwkv_k_k[i], rwkv_k_a[i], rwkv_r_k[i], rwkv_gn_g[i], rwkv_gn_b[i])
        y_b = _dilated_attention(z_dq, z_dk, z_dv)
        y_c = _mla(z_qa, z_kva, z_kr, mla_qa_g[i], mla_w_uq[i], mla_kva_g[i], mla_w_ukv[i])
        mix = jnp.concatenate([y_a, y_b, y_c], axis=-1) @ w_out[i]
        x = _layernorm(DEEPNORM_ALPHA * x + mix, ln1_g[i], ln1_b[i])
        ffn = _moe(x, router_w, router_b, moe_w_gate[i], moe_w_up[i], moe_w_down[i])
        x = _layernorm(DEEPNORM_ALPHA * x + ffn, ln2_g[i], ln2_b[i])
        x = x + jax.nn.sigmoid(x @ ple_w_gate[i]) * (p[i] @ ple_w_proj[i])
    return x
```

```python
import numpy as np
from contextlib import ExitStack
import concourse.bass as bass
import concourse.mybir as mybir
from concourse.bass_utils import run_bass_kernel_spmd

F32 = mybir.dt.float32
BF16 = mybir.dt.bfloat16
AF = mybir.ActivationFunctionType
ALU = mybir.AluOpType
AX = mybir.AxisListType

NCORES = 2
D = 2048
B = 2
S = 4096
DEPTH = 4
IN_DIM = 8000
ALPHA = (2 * DEPTH) ** 0.25
LN_EPS = 1e-5
RMS_EPS = 1e-6
NDS = 24


class View:
    __slots__ = ("buf", "ap")

    def __init__(self, buf, ap):
        self.buf = buf
        self.ap = ap

    def __getitem__(self, idx):
        return View(self.buf, self.ap[idx])


class Buf:
    def __init__(self, t, name):
        self.t = t
        self.name = name
        self.w = None
        self.r = {}

    def __getitem__(self, idx):
        return View(self, self.t[idx])

    def v(self, fn):
        return View(self, fn(self.t))


class KB:
    def __init__(self):
        self.nc = bass.Bass("TRN2", target_bir_lowering=False)
        self.es = ExitStack()
        nc = self.nc
        self.eng = {"pe": nc.tensor, "dve": nc.vector, "act": nc.scalar, "pool": nc.gpsimd, "sp": nc.sync}
        self.sem = {}
        self.cnt = {}
        for k in ("pe", "dve", "act", "pool"):
            self.sem[k] = self.es.enter_context(nc.semaphore("s_" + k))
            self.cnt[k] = 0
        self.seen = {k: {} for k in self.eng}
        self.dsem = [self.es.enter_context(nc.semaphore("d%d" % i)) for i in range(NDS)]
        self.dcnt = [0] * NDS
        self.dnext = 0
        self.out_events = []
        self.nuid = 0

    def uid(self, p):
        self.nuid += 1
        return "%s_%d" % (p, self.nuid)

    def sb(self, shape, dt, name="sb", es=None):
        nm = self.uid(name)
        t = (es or self.es).enter_context(self.nc.sbuf_tensor(nm, list(shape), dt))
        return Buf(t, nm)

    def ps(self, shape, dt=F32, name="ps", es=None):
        nm = self.uid(name)
        t = (es or self.es).enter_context(self.nc.psum_tensor(nm, list(shape), dt))
        return Buf(t, nm)

    def dram(self, name, shape, dt, kind):
        t = self.nc.dram_tensor(name, list(shape), dt, kind=kind).ap()
        return Buf(t, name)

    def _wait(self, e, ev):
        sem, val, src = ev
        if src == "pe" and e == "pe":
            return
        key = id(sem)
        if self.seen[e].get(key, 0) >= val:
            return
        self.seen[e][key] = val
        self.eng[e].wait_ge(sem, val)

    def _deps(self, e, reads, writes):
        for b in reads:
            if b.w is not None:
                self._wait(e, b.w)
        for b in writes:
            if b.w is not None:
                self._wait(e, b.w)
            for ev in b.r.values():
                self._wait(e, ev)

    def _commit(self, ev, reads, writes):
        for b in reads:
            if b not in writes:
                b.r[id(ev[0])] = ev
        for b in writes:
            b.w = ev
            b.r = {}

    def op(self, e, fn, *args, **kw):
        reads, writes = [], []
        kk = {}
        for k, v in kw.items():
            if isinstance(v, View):
                (writes if (k.startswith("out") or k == "accum_out") else reads).append(v.buf)
                kk[k] = v.ap
            else:
                kk[k] = v
        self._deps(e, reads, writes)
        ins = getattr(self.eng[e], fn)(*args, **kk)
        self.cnt[e] += 1
        ins.then_inc(self.sem[e], 1)
        ev = (self.sem[e], self.cnt[e], e)
        self._commit(ev, reads, writes)
        return ins

    def memset(self, view, val, e="dve"):
        self._deps(e, [], [view.buf])
        ins = self.eng[e].memset(view.ap, val)
        self.cnt[e] += 1
        ins.then_inc(self.sem[e], 1)
        self._commit((self.sem[e], self.cnt[e], e), [], [view.buf])

    def dma(self, q, out, in_, is_output=False):
        reads, writes = [], []
        if isinstance(out, View):
            writes.append(out.buf)
            out = out.ap
        if isinstance(in_, View):
            reads.append(in_.buf)
            in_ = in_.ap
        self._deps(q, reads, writes)
        i = self.dnext
        self.dnext = (self.dnext + 1) % NDS
        if self.dcnt[i] > 0:
            self._wait(q, (self.dsem[i], 16 * self.dcnt[i], "dma"))
        ins = self.eng[q].dma_start(out=out, in_=in_)
        self.dcnt[i] += 1
        ins.then_inc(self.dsem[i], 16)
        ev = (self.dsem[i], 16 * self.dcnt[i], "dma")
        self._commit(ev, reads, writes)
        if is_output:
            self.out_events.append(ev)
        return ins

    def barrier(self):
        for e in self.eng:
            for k in ("pe", "dve", "act", "pool"):
                if k != e and self.cnt[k] > 0:
                    self._wait(e, (self.sem[k], self.cnt[k], k))
            for i in range(NDS):
                if self.dcnt[i] > 0:
                    self._wait(e, (self.dsem[i], 16 * self.dcnt[i], "dma"))

    def finish(self):
        for ev in self.out_events:
            self._wait("sp", ev)
        self.es.close()
        return self.nc


def mm(kb, out, lhsT, rhs, start, stop):
    kb.op("pe", "matmul", out=out, lhsT=lhsT, rhs=rhs, start=start, stop=stop)


def mm(kb, out, lhsT, rhs, start, stop):
    kb.op("pe", "matmul", out=out, lhsT=lhsT, rhs=rhs, start=start, stop=stop)


def rsqrt_eps(kb, out, in_, eps):
    kb.op("act", "activation", out=out, in_=in_, func=AF.Ln, bias=float(eps))
    kb.op("act", "activation", out=out, in_=out, func=AF.Exp, scale=-0.5)


def dview(buf, pattern, **kw):
    return View(buf, buf.t.rearrange(pattern, **kw))


HOFF = 2560
DQ0, DK0, DV0 = 2560, 4096, 5632
QA0, KVA0, KR0 = 7168, 7680, 7936
ZROWS = 8064


def emit_inproj(kb, xT, w_in, zT, zv):
    es = ExitStack()
    NB = 512
    TH = 2048
    xbf = kb.sb([128, 16, TH], BF16, "xbf", es)
    wbufs = [kb.sb([128, 16, NB], BF16, "winb", es) for _ in range(2)]
    pss = [kb.ps([128, 512], F32, "pz", es) for _ in range(4)]
    pvs = [kb.ps([128, 512], F32, "pv", es) for _ in range(2)]
    zsb = [kb.sb([128, TH], F32, "zsb", es) for _ in range(2)]
    vsb = [kb.sb([128, 512], F32, "vsb", es) for _ in range(2)]
    wv = w_in.t.rearrange("(c p) n -> p c n", p=128)
    xv = xT.t.rearrange("(c p) t -> p c t", p=128)
    ib = im = ip = iv = 0
    for half in range(S // TH):
        h0 = half * TH
        for c0 in range(0, 16, 4):
            kb.dma("pool", xbf[:, c0:c0 + 4, :], View(xT, xv[:, c0:c0 + 4, h0:h0 + TH]))
        for nb in range(0, IN_DIM, NB):
            ncols = min(NB, IN_DIM - nb)
            wb = wbufs[ib % 2]
            ib += 1
            for c0 in range(0, 16, 8):
                kb.dma("pool", wb[:, c0:c0 + 8, 0:ncols], View(w_in, wv[:, c0:c0 + 8, nb:nb + ncols]))
            for m0 in range(0, ncols, 128):
                msz = min(128, ncols - m0)
                zs = zsb[im % 2]
                im += 1
                for th in range(TH // 512):
                    ps = pss[ip % 4]
                    ip += 1
                    for kc in range(16):
                        mm(kb, ps[0:msz, :], wb[:, kc, m0:m0 + msz], xbf[:, kc, th * 512:(th + 1) * 512],
                           kc == 0, kc == 15)
                    if th % 2 == 0:
                        kb.op("act", "copy", out=zs[0:msz, th * 512:(th + 1) * 512], in_=ps[0:msz, :])
                    else:
                        kb.op("dve", "tensor_copy", out=zs[0:msz, th * 512:(th + 1) * 512], in_=ps[0:msz, :])
                kb.dma("sp", View(zT, zT.t[nb + m0:nb + m0 + msz, h0:h0 + TH]), zs[0:msz, :])
            if DV0 <= nb < DV0 + 1536:
                for tt in range(TH // 128):
                    ps = pvs[iv % 2]
                    vs = vsb[iv % 2]
                    iv += 1
                    for kc in range(16):
                        mm(kb, ps[:, :], xbf[:, kc, tt * 128:(tt + 1) * 128], wb[:, kc, :], kc == 0, kc == 15)
                    if tt % 2 == 0:
                        kb.op("act", "copy", out=vs[:, :], in_=ps[:, :])
                    else:
                        kb.op("dve", "tensor_copy", out=vs[:, :], in_=ps[:, :])
                    kb.dma("sp", View(zv, zv.t[h0 + tt * 128:h0 + (tt + 1) * 128, nb - DV0:nb - DV0 + 512]), vs[:, :])
    kb.barrier()
    es.close()


def emit_ln(kb, h, NT, onesf, g, b, gi, pst, tmp, stat):
    pm, pq = pst
    for c in range(16):
        mm(kb, pm[:, :], onesf[:, :], h[:, c, :], c == 0, c == 15)
    for c in range(16):
        kb.op("act", "activation", out=tmp[:, :], in_=h[:, c, :], func=AF.Square)
        mm(kb, pq[:, :], onesf[:, :], tmp[:, :], c == 0, c == 15)
    mean = stat[:, 0, :]
    rstd = stat[:, 1, :]
    kb.op("act", "copy", out=mean, in_=pm[:, :])
    kb.op("dve", "tensor_tensor", out=tmp[:, :], in0=mean, in1=mean, op=ALU.mult)
    kb.op("dve", "tensor_tensor", out=rstd, in0=pq[:, :], in1=tmp[:, :], op=ALU.subtract)
    rsqrt_eps(kb, rstd, rstd, LN_EPS)
    for c in range(16):
        kb.op("dve", "tensor_tensor", out=h[:, c, :], in0=h[:, c, :], in1=mean, op=ALU.subtract)
        kb.op("dve", "tensor_tensor", out=h[:, c, :], in0=h[:, c, :], in1=rstd, op=ALU.mult)
        kb.op("act", "activation", out=h[:, c, :], in_=h[:, c, :], func=AF.Identity,
              scale=g[:, gi + c:gi + c + 1], bias=b[:, gi + c:gi + c + 1])


def emit_outproj(kb, l, mixT, w_out, xT, x1T, x1b, gates, cst):
    es = ExitStack()
    NT = 256
    wb = kb.sb([128, 16, 2048], BF16, "woutb", es)
    wv = w_out.t.rearrange("(c p) n -> p c n", p=128)
    for c0 in range(0, 16, 4):
        kb.dma("pool", wb[:, c0:c0 + 4, :], View(w_out, wv[:, c0:c0 + 4, :]))
    mxs = [kb.sb([128, 16, NT], BF16, "mixsb", es) for _ in range(2)]
    xs = kb.sb([128, 16, NT], F32, "xsb", es)
    h = kb.sb([128, 16, NT], F32, "hsb", es)
    hb = kb.sb([128, 16, NT], BF16, "hbsb", es)
    tmp = kb.sb([128, NT], F32, "lntmp", es)
    stat = kb.sb([128, 2, NT], F32, "lnstat", es)
    pso = [kb.ps([128, NT], F32, "po", es) for _ in range(3)]
    pst = [kb.ps([128, NT], F32, "pst", es) for _ in range(2)]
    prt = [kb.ps([128, 32], F32, "prt", es) for _ in range(2)]
    pgt = kb.ps([32, 128], F32, "pgt", es)
    gts = kb.sb([32, 128], F32, "gts", es)
    rs = [kb.sb([128, 32], F32, "rs%d" % i, es) for i in range(8)]
    r8 = [kb.sb([128, 8], F32, "r8%d" % i, es) for i in range(4)]
    r1 = [kb.sb([128, 1], F32, "r1%d" % i, es) for i in range(2)]
    mv = mixT.t.rearrange("(c p) t -> p c t", p=128)
    xv = xT.t.rearrange("(c p) t -> p c t", p=128)
    x1v = x1T.t.rearrange("(c p) t -> p c t", p=128)
    x1bv = x1b.t.rearrange("(c p) t -> p c t", p=128)
    for it in range(S // NT):
        t0 = it * NT
        mx = mxs[it % 2]
        kb.dma("sp", mx[:, :, :], View(mixT, mv[:, :, t0:t0 + NT]))
        kb.dma("sp", xs[:, :, :], View(xT, xv[:, :, t0:t0 + NT]))
        for oc in range(16):
            ps = pso[oc % 3]
            for kc in range(16):
                mm(kb, ps[:, :], wb[:, kc, oc * 128:(oc + 1) * 128], mx[:, kc, :], kc == 0, kc == 15)
            kb.op("dve", "scalar_tensor_tensor", out=h[:, oc, :], in0=xs[:, oc, :], scalar=ALPHA, in1=ps[:, :],
                  op0=ALU.mult, op1=ALU.add)
        emit_ln(kb, h, NT, cst["onesf"], cst["ln1g"], cst["ln1b"], l * 16, pst, tmp, stat)
        kb.dma("sp", View(x1T, x1v[:, :, t0:t0 + NT]), h[:, :, :])
        for c in range(16):
            if c % 2 == 0:
                kb.op("act", "copy", out=hb[:, c, :], in_=h[:, c, :])
            else:
                kb.op("pool", "tensor_copy", out=hb[:, c, :], in_=h[:, c, :])
        kb.dma("sp", View(x1b, x1bv[:, :, t0:t0 + NT]), hb[:, :, :])
        for sub in range(NT // 128):
            pr = prt[sub % 2]
            for kc in range(16):
                mm(kb, pr[:, :], h[:, kc, sub * 128:(sub + 1) * 128], cst["rw"][:, kc, :], kc == 0, kc == 15)
            sc, sel, eq1, sel2, eq2, gt, msk, gout = rs
            m1, m2, gs, gsel = r8
            gmax, den = r1
            kb.op("act", "activation", out=sc[:, :], in_=pr[:, :], func=AF.Sigmoid)
            kb.op("dve", "tensor_tensor", out=sel[:, :], in0=sc[:, :], in1=cst["rb"][:, :], op=ALU.add)
            g3 = lambda v: View(v.buf, v.ap.rearrange("p (g e) -> p g e", e=4))
            b3 = lambda v: View(v.buf, v.ap.unsqueeze(2).to_broadcast([128, 8, 4]))
            kb.op("dve", "tensor_reduce", out=m1[:, :], in_=g3(sel[:, :]), axis=AX.X, op=ALU.max)
            kb.op("dve", "tensor_tensor", out=g3(eq1[:, :]), in0=g3(sel[:, :]), in1=b3(m1[:, :]), op=ALU.is_equal)
            kb.op("dve", "scalar_tensor_tensor", out=sel2[:, :], in0=eq1[:, :], scalar=-1.0e4, in1=sel[:, :],
                  op0=ALU.mult, op1=ALU.add)
            kb.op("dve", "tensor_reduce", out=m2[:, :], in_=g3(sel2[:, :]), axis=AX.X, op=ALU.max)
            kb.op("dve", "tensor_tensor", out=g3(eq2[:, :]), in0=g3(sel2[:, :]), in1=b3(m2[:, :]), op=ALU.is_equal)
            kb.op("dve", "tensor_tensor", out=gs[:, :], in0=m1[:, :], in1=m2[:, :], op=ALU.add)
            kb.op("dve", "tensor_reduce", out=gmax[:, :], in_=gs[:, :], axis=AX.X, op=ALU.max)
            kb.op("dve", "tensor_scalar", out=gsel[:, :], in0=gs[:, :], scalar1=gmax[:, 0:1], scalar2=None,
                  op0=ALU.is_equal)
            kb.op("dve", "tensor_tensor", out=msk[:, :], in0=eq1[:, :], in1=eq2[:, :], op=ALU.add)
            kb.op("dve", "tensor_tensor", out=g3(msk[:, :]), in0=g3(msk[:, :]), in1=b3(gsel[:, :]), op=ALU.mult)
            kb.op("dve", "tensor_tensor", out=gt[:, :], in0=msk[:, :], in1=sc[:, :], op=ALU.mult)
            kb.op("dve", "tensor_reduce", out=den[:, :], in_=gt[:, :], axis=AX.X, op=ALU.add)
            kb.op("dve", "reciprocal", out=den[:, :], in_=den[:, :])
            kb.op("dve", "tensor_scalar", out=gout[:, :], in0=gt[:, :], scalar1=den[:, 0:1], scalar2=None,
                  op0=ALU.mult)
            kb.op("pe", "transpose", out=pgt[:, :], in_=gout[:, :], identity=cst["ident"][:, :])
            kb.op("act", "copy", out=gts[:, :], in_=pgt[:, :])
            kb.dma("sp", View(gates, gates.t[:, t0 + sub * 128:t0 + (sub + 1) * 128]), gts[:, :])
    kb.barrier()
    es.close()


def emit_moe(kb, x1b, gates, wg, wu, wd, ffnT, cst, wsc):
    es = ExitStack()
    NT = 512
    xs = kb.sb([128, 16, NT], BF16, "mx", es)
    grow = kb.sb([32, NT], F32, "grow", es)
    rowsel = kb.sb([32, 32, 128], F32, "rowsel", es)
    kb.dma("sp", rowsel[:, :, :], View(cst["rowsel_d"], cst["rowsel_d"].t.rearrange("k (e p) -> k e p", p=128)))
    y = kb.sb([128, 16, NT], F32, "my", es)
    wgb = [kb.sb([128, 16, 512], BF16, "wgb", es) for _ in range(2)]
    wub = [kb.sb([128, 16, 512], BF16, "wub", es) for _ in range(2)]
    wdb = [kb.sb([128, 4, 2048], BF16, "wdb", es) for _ in range(2)]
    hg = [kb.sb([128, 4, NT], BF16, "hg", es) for _ in range(2)]
    sg = [kb.sb([128, NT], F32, "sg", es) for _ in range(2)]
    pg = [kb.ps([128, NT], F32, "pg", es) for _ in range(2)]
    pu = [kb.ps([128, NT], F32, "pu", es) for _ in range(2)]
    pd = [kb.ps([128, NT], F32, "pd", es) for _ in range(3)]
    pb = kb.ps([128, NT], F32, "pb", es)
    xv = x1b.t.rearrange("(c p) t -> p c t", p=128)
    fv = ffnT.t.rearrange("(c p) t -> p c t", p=128)
    ie = 0
    for it in range(S // NT):
        t0 = it * NT
        kb.dma("sp", xs[:, :, :], View(x1b, xv[:, :, t0:t0 + NT]))
        kb.dma("sp", grow[:, :], View(gates, gates.t[:, t0:t0 + NT]))
        for e in range(32):
            b = ie % 2
            ie += 1
            if it == 0:
                kb.dma("pool", wgb[b][:, :, :], View(wg, wg.t[e].rearrange("(c p) n -> p c n", p=128)))
                kb.dma("pool", wub[b][:, :, :], View(wu, wu.t[e].rearrange("(c p) n -> p c n", p=128)))
                kb.dma("pool", wdb[b][:, :, :], View(wd, wd.t[e].rearrange("(c p) n -> p c n", p=128)))
                kb.dma("sp", View(wsc[e][0], wsc[e][0].t), wgb[b][:, :, :])
                kb.dma("sp", View(wsc[e][1], wsc[e][1].t), wub[b][:, :, :])
                kb.dma("sp", View(wsc[e][2], wsc[e][2].t), wdb[b][:, :, :])
            else:
                kb.dma("sp", wgb[b][:, :, :], View(wsc[e][0], wsc[e][0].t))
                kb.dma("act", wub[b][:, :, :], View(wsc[e][1], wsc[e][1].t))
                kb.dma("sp", wdb[b][:, :, :], View(wsc[e][2], wsc[e][2].t))
            hgt = hg[b]
            mm(kb, pb[:, :], rowsel[0:32, e, :], grow[0:32, :], True, True)
            for hc in range(4):
                p1 = pg[hc % 2]
                p2 = pu[hc % 2]
                s1 = sg[hc % 2]
                for kc in range(16):
                    mm(kb, p1[:, :], wgb[b][:, kc, hc * 128:(hc + 1) * 128], xs[:, kc, :], kc == 0, kc == 15)
                for kc in range(16):
                    mm(kb, p2[:, :], wub[b][:, kc, hc * 128:(hc + 1) * 128], xs[:, kc, :], kc == 0, kc == 15)
                kb.op("act", "activation", out=s1[:, :], in_=p1[:, :], func=AF.Silu)
                kb.op("dve", "tensor_tensor", out=s1[:, :], in0=s1[:, :], in1=p2[:, :], op=ALU.mult)
                kb.op("dve", "tensor_tensor", out=hgt[:, hc, :], in0=s1[:, :], in1=pb[:, :], op=ALU.mult)
            for oc in range(16):
                ps = pd[oc % 3]
                for hc in range(4):
                    mm(kb, ps[:, :], wdb[b][:, hc, oc * 128:(oc + 1) * 128], hgt[:, hc, :], hc == 0, hc == 3)
                if e == 0:
                    kb.op("act", "copy", out=y[:, oc, :], in_=ps[:, :])
                else:
                    kb.op("dve", "tensor_tensor", out=y[:, oc, :], in0=y[:, oc, :], in1=ps[:, :], op=ALU.add)
        kb.dma("sp", View(ffnT, fv[:, :, t0:t0 + NT]), y[:, :, :])
    kb.barrier()
    es.close()


def emit_ple(kb, l, x1T, ffnT, pT, wpg, wpp, xoT, cst):
    es = ExitStack()
    NT = 256
    wb = kb.sb([128, 16, 2048], BF16, "wpgb", es)
    wv = wpg.t.rearrange("(c p) n -> p c n", p=128)
    for c0 in range(0, 16, 4):
        kb.dma("pool", wb[:, c0:c0 + 4, :], View(wpg, wv[:, c0:c0 + 4, :]))
    wpb = kb.sb([128, 2, 2048], BF16, "wppb", es)
    kb.dma("pool", wpb[:, :, :], View(wpp, wpp.t.rearrange("(c p) n -> p c n", p=128)))
    h = kb.sb([128, 16, NT], F32, "gh", es)
    f = kb.sb([128, 16, NT], F32, "gf", es)
    hb = kb.sb([128, 16, NT], BF16, "ghb", es)
    pb = kb.sb([128, 2, NT], BF16, "gpb", es)
    tmp = kb.sb([128, NT], F32, "gtmp", es)
    stat = kb.sb([128, 2, NT], F32, "gstat", es)
    sgs = [kb.sb([128, NT], F32, "gsg", es) for _ in range(2)]
    pst = [kb.ps([128, NT], F32, "gpst", es) for _ in range(2)]
    pgs = [kb.ps([128, NT], F32, "gpg", es) for _ in range(2)]
    pps = [kb.ps([128, NT], F32, "gpp", es) for _ in range(2)]
    x1v = x1T.t.rearrange("(c p) t -> p c t", p=128)
    fv = ffnT.t.rearrange("(c p) t -> p c t", p=128)
    ov = xoT.t.rearrange("(c p) t -> p c t", p=128)
    pv = pT.t.rearrange("(c p) t -> p c t", p=128)
    for it in range(S // NT):
        t0 = it * NT
        kb.dma("sp", h[:, :, :], View(x1T, x1v[:, :, t0:t0 + NT]))
        kb.dma("sp", f[:, :, :], View(ffnT, fv[:, :, t0:t0 + NT]))
        kb.dma("pool", pb[:, :, :], View(pT, pv[:, :, t0:t0 + NT]))
        for c in range(16):
            kb.op("dve", "scalar_tensor_tensor", out=h[:, c, :], in0=h[:, c, :], scalar=ALPHA, in1=f[:, c, :],
                  op0=ALU.mult, op1=ALU.add)
        emit_ln(kb, h, NT, cst["onesf"], cst["ln2g"], cst["ln2b"], l * 16, pst, tmp, stat)
        for c in range(16):
            if c % 2 == 0:
                kb.op("act", "copy", out=hb[:, c, :], in_=h[:, c, :])
            else:
                kb.op("pool", "tensor_copy", out=hb[:, c, :], in_=h[:, c, :])
        for oc in range(16):
            p1 = pgs[oc % 2]
            p2 = pps[oc % 2]
            s1 = sgs[oc % 2]
            for kc in range(16):
                mm(kb, p1[:, :], wb[:, kc, oc * 128:(oc + 1) * 128], hb[:, kc, :], kc == 0, kc == 15)
            for kc in range(2):
                mm(kb, p2[:, :], wpb[:, kc, oc * 128:(oc + 1) * 128], pb[:, kc, :], kc == 0, kc == 1)
            kb.op("act", "activation", out=s1[:, :], in_=p1[:, :], func=AF.Sigmoid)
            kb.op("dve", "tensor_tensor", out=s1[:, :], in0=s1[:, :], in1=p2[:, :], op=ALU.mult)
            kb.op("dve", "tensor_tensor", out=f[:, oc, :], in0=h[:, oc, :], in1=s1[:, :], op=ALU.add)
        kb.dma("sp", View(xoT, ov[:, :, t0:t0 + NT]), f[:, :, :], is_output=True)
    kb.barrier()
    es.close()


DIL = (1, 4, 16)


def dil_consts():
    out = np.zeros((128, 4, 3, 2, 128), np.float32)
    k = np.arange(128)[:, None]
    q = np.arange(128)[None, :]
    for slot in range(4):
        for g, d in enumerate(DIL):
            head = g * 4 + slot
            slope = 2.0 ** (-8.0 * (head + 1) / 12.0)
            out[:, slot, g, 0, :] = np.where(k >= q, -slope * d * (q - k + 128), -30000.0)
            out[:, slot, g, 1, :] = np.where(k <= q, -slope * d * (q - k), -30000.0)
    return out.reshape(128, 4 * 3 * 2 * 128)


def emit_dil(kb, zT, zv, mixT, cst):
    es = ExitStack()
    bias = kb.sb([128, 4, 3, 2, 128], F32, "dbias", es)
    kb.dma("sp", bias[:, :, :, :, :], View(cst["dbias_d"], cst["dbias_d"].t.rearrange("p (s g h q) -> p s g h q", s=4, g=3, h=2)))
    onesb = cst["onesb"]
    qT = kb.sb([128, S], BF16, "dqT", es)
    kT = kb.sb([128, S], BF16, "dkT", es)
    vb = [kb.sb([128, 128], BF16, "dvb", es) for _ in range(3)]
    num = kb.sb([128, S], F32, "dnum", es)
    den = kb.sb([128, S], F32, "dden", es)
    yb = kb.sb([128, S], BF16, "dyb", es)
    sps = [kb.ps([128, 256], F32, "dsp", es) for _ in range(2)]
    ops = [kb.ps([128, 128], F32, "dop", es) for _ in range(2)]
    dps = [kb.ps([128, 128], F32, "ddp", es) for _ in range(2)]
    tsb = [kb.sb([128, 256], F32, "dts", es) for _ in range(2)]
    pT = [kb.sb([128, 256], BF16, "dpT", es) for _ in range(2)]
    scale = 128.0 ** -0.5
    for slot in range(4):
        for g, d in enumerate(DIL):
            head = g * 4 + slot
            nbs = 32 // d
            kb.dma("pool", qT[:, :], View(zT, zT.t[DQ0 + head * 128:DQ0 + (head + 1) * 128, :]))
            kb.dma("pool", kT[:, :], View(zT, zT.t[DK0 + head * 128:DK0 + (head + 1) * 128, :]))
            for n in range(32):
                r, m = divmod(n, nbs)
                has_prev = m > 0
                t0 = r + d * 128 * m
                tok = slice(t0, t0 + d * 127 + 1, d)
                tokp = slice(t0 - d * 128, t0 - d * 128 + d * 127 + 1, d)
                vcur = vb[n % 3]
                kb.dma("pool", vcur[:, :], View(zv, zv.t[tok, head * 128:(head + 1) * 128]))
                vprev = vb[(n - 1) % 3]
                sp = sps[n % 2]
                ts = tsb[n % 2]
                pt = pT[n % 2]
                op_ = ops[n % 2]
                dp_ = dps[n % 2]
                lo = 0 if has_prev else 1
                if has_prev:
                    mm(kb, sp[:, 0:128], View(kT, kT.t[:, tokp]), View(qT, qT.t[:, tok]), True, True)
                mm(kb, sp[:, 128:256], View(kT, kT.t[:, tok]), View(qT, qT.t[:, tok]), True, True)
                bv = View(bias, bias.t[:, slot, g, lo:2, :].rearrange("p h q -> p (h q)"))
                kb.op("dve", "scalar_tensor_tensor", out=ts[:, lo * 128:256], in0=sp[:, lo * 128:256], scalar=scale,
                      in1=bv, op0=ALU.mult, op1=ALU.add)
                kb.op("act", "activation", out=pt[:, lo * 128:256], in_=ts[:, lo * 128:256], func=AF.Exp)
                if has_prev:
                    mm(kb, op_[:, :], vprev[:, :], pt[:, 0:128], True, False)
                mm(kb, op_[:, :], vcur[:, :], pt[:, 128:256], not has_prev, True)
                if has_prev:
                    mm(kb, dp_[:, :], onesb[:, :], pt[:, 0:128], True, False)
                mm(kb, dp_[:, :], onesb[:, :], pt[:, 128:256], not has_prev, True)
                nv = View(num, num.t[:, tok])
                dv = View(den, den.t[:, tok])
                if g == 0:
                    kb.op("act", "copy", out=nv, in_=op_[:, :])
                    kb.op("dve", "tensor_copy", out=dv, in_=dp_[:, :])
                else:
                    kb.op("dve", "tensor_tensor", out=nv, in0=nv, in1=op_[:, :], op=ALU.add)
                    kb.op("dve", "tensor_tensor", out=dv, in0=dv, in1=dp_[:, :], op=ALU.add)
        kb.op("dve", "reciprocal", out=den[:, :], in_=den[:, :])
        kb.op("dve", "tensor_tensor", out=yb[:, :], in0=num[:, :], in1=den[:, :], op=ALU.mult)
        kb.dma("sp", View(mixT, mixT.t[768 + slot * 128:768 + (slot + 1) * 128, :]), yb[:, :])
    kb.barrier()
    es.close()


def rope_consts():
    half = 32
    inv = 10000.0 ** (-np.arange(half, dtype=np.float32) / half)
    ang = np.arange(S, dtype=np.float32)[None, :] * inv[:, None]
    cos = np.concatenate([np.cos(ang), np.cos(ang)], 0)
    sin = np.concatenate([-np.sin(ang), np.sin(ang)], 0)
    return np.stack([cos, sin], 1).astype(np.float32).reshape(64, 2 * S)


def mla_masks():
    k = np.arange(128)[:, None, None] + 128 * np.arange(4)[None, :, None]
    q = np.arange(512)[None, None, :]
    return np.where(k <= q, 0.0, -30000.0).astype(np.float32).reshape(128, 4 * 512)


def emit_rms(kb, zt, nch, NT, gam, ones_sc, ps, tmp, out_bf):
    for c in range(nch):
        kb.op("act", "activation", out=tmp[:, :], in_=zt[:, c, :], func=AF.Square)
        mm(kb, ps[:, :], ones_sc[:, :], tmp[:, :], c == 0, c == nch - 1)
    rsqrt_eps(kb, tmp[:, :], ps[:, :], RMS_EPS)
    for c in range(nch):
        kb.op("dve", "scalar_tensor_tensor", out=out_bf[:, c, :], in0=zt[:, c, :], scalar=gam[:, c:c + 1], in1=tmp[:, :],
              op0=ALU.mult, op1=ALU.mult)


def emit_mla(kb, l, zT, wuq, wukv, mixT, cst, scr):
    es = ExitStack()
    NT = 512
    masks = kb.sb([128, 4, 512], F32, "mmask", es)
    kb.dma("sp", masks[:, :, :], View(cst["mmask_d"], cst["mmask_d"].t.rearrange("p (a q) -> p a q", a=4)))
    onesb = cst["onesb"]
    wkv = kb.sb([128, 2, 1536], BF16, "wkv", es)
    kb.dma("pool", wkv[:, :, :], View(wukv, wukv.t.rearrange("(c p) n -> p c n", p=128)))
    kvn = kb.sb([128, 2, S], BF16, "kvn", es)
    krT = kb.sb([64, S], BF16, "krT", es)
    psq = [kb.ps([128, NT], F32, "mpsq", es) for _ in range(2)]
    es1 = ExitStack()
    rope = kb.sb([64, 2, S], F32, "rope", es1)
    kb.dma("sp", rope[:, :, :], View(cst["rope_d"], cst["rope_d"].t.rearrange("p (a t) -> p a t", a=2)))
    wq = kb.sb([128, 4, 1152], BF16, "wq", es1)
    kb.dma("pool", wq[:, :, :], View(wuq, wuq.t.rearrange("(c p) n -> p c n", p=128)))
    wqs = kb.sb([128, 4, 6, 64], BF16, "wqs", es1)
    wq4 = wuq.t.rearrange("(c p) (h e) -> p c h e", p=128, e=192)
    for c in range(4):
        kb.dma("pool", wqs[:, c, :, 0:32], View(wuq, wq4[:, c, :, 160:192]))
        kb.dma("pool", wqs[:, c, :, 32:64], View(wuq, wq4[:, c, :, 128:160]))
    zt = kb.sb([128, 4, NT], F32, "mzt", es1)
    zb = kb.sb([128, 4, NT], BF16, "mzb", es1)
    tmp = kb.sb([128, NT], F32, "mtmp", es1)
    t64 = [kb.sb([64, NT], F32, "mt64", es1) for _ in range(3)]
    qst = [kb.sb([128, NT], BF16, "mqst", es1) for _ in range(2)]
    qrs = [kb.sb([64, NT], BF16, "mqrs", es1) for _ in range(2)]
    ps1 = kb.ps([128, NT], F32, "mps1", es1)
    psr = [kb.ps([64, NT], F32, "mpsr", es1) for _ in range(2)]
    qn_d, qr_d = scr["qn"], scr["qr"]
    for it in range(S // NT):
        t0 = it * NT
        kb.dma("sp", zt[:, :, :], View(zT, zT.t[QA0:QA0 + 512, t0:t0 + NT].rearrange("(c p) t -> p c t", p=128)))
        emit_rms(kb, zt, 4, NT, View(cst["qag"], cst["qag"].t[:, l * 4:(l + 1) * 4]), cst["ones512"], ps1, tmp, zb)
        for hd in range(6):
            pq = psq[hd % 2]
            for c in range(4):
                mm(kb, pq[:, :], wq[:, c, hd * 192:hd * 192 + 128], zb[:, c, :], c == 0, c == 3)
            qs = qst[hd % 2]
            kb.op("act", "copy", out=qs[:, :], in_=pq[:, :])
            kb.dma("sp", View(qn_d, qn_d.t[hd, :, t0:t0 + NT]), qs[:, :])
            pr, pw = psr
            for c in range(4):
                mm(kb, pr[:, :], wq[:, c, hd * 192 + 128:hd * 192 + 192], zb[:, c, :], c == 0, c == 3)
            for c in range(4):
                mm(kb, pw[:, :], wqs[:, c, hd, :], zb[:, c, :], c == 0, c == 3)
            a, b2, _ = t64
            kb.op("dve", "tensor_tensor", out=a[:, :], in0=pr[:, :], in1=View(rope, rope.t[:, 0, t0:t0 + NT]), op=ALU.mult)
            kb.op("dve", "tensor_tensor", out=b2[:, :], in0=pw[:, :], in1=View(rope, rope.t[:, 1, t0:t0 + NT]), op=ALU.mult)
            qr_ = qrs[hd % 2]
            kb.op("dve", "tensor_tensor", out=qr_[:, :], in0=a[:, :], in1=b2[:, :], op=ALU.add)
            kb.dma("sp", View(qr_d, qr_d.t[hd, :, t0:t0 + NT]), qr_[:, :])
        kb.dma("sp", zt[:, 0:2, :], View(zT, zT.t[KVA0:KVA0 + 256, t0:t0 + NT].rearrange("(c p) t -> p c t", p=128)))
        emit_rms(kb, zt, 2, NT, View(cst["kvag"], cst["kvag"].t[:, l * 2:(l + 1) * 2]), cst["ones256"], ps1, tmp,
                 View(kvn, kvn.t[:, :, t0:t0 + NT]))
        a, b2, c2 = t64
        kb.dma("sp", a[:, :], View(zT, zT.t[KR0:KR0 + 64, t0:t0 + NT]))
        kb.dma("sp", b2[0:32, :], View(zT, zT.t[KR0 + 32:KR0 + 64, t0:t0 + NT]))
        kb.dma("sp", b2[32:64, :], View(zT, zT.t[KR0:KR0 + 32, t0:t0 + NT]))
        kb.op("dve", "tensor_tensor", out=a[:, :], in0=a[:, :], in1=View(rope, rope.t[:, 0, t0:t0 + NT]), op=ALU.mult)
        kb.op("dve", "tensor_tensor", out=b2[:, :], in0=b2[:, :], in1=View(rope, rope.t[:, 1, t0:t0 + NT]), op=ALU.mult)
        kb.op("dve", "tensor_tensor", out=View(krT, krT.t[:, t0:t0 + NT]), in0=a[:, :], in1=b2[:, :], op=ALU.add)
    kb.barrier()
    es1.close()
    knT = kb.sb([128, S], BF16, "knT", es)
    vtm = kb.sb([128, 32, 128], BF16, "vtm", es)
    qn = kb.sb([128, S], BF16, "qnT", es)
    qr = kb.sb([64, S], BF16, "qrT", es)
    pts = [kb.sb([128, NT], BF16, "mpt", es) for _ in range(2)]
    tss = [kb.sb([128, NT], F32, "mts", es) for _ in range(2)]
    ysb = kb.sb([128, NT], F32, "mys", es)
    ybf = [kb.sb([128, NT], BF16, "mybf", es) for _ in range(2)]
    pss = [kb.ps([128, NT], F32, "mpss", es) for _ in range(2)]
    pso = kb.ps([128, NT], F32, "mpso", es)
    psd = kb.ps([128, NT], F32, "mpsd", es)
    scale = 192.0 ** -0.5
    ic = 0
    for hd in range(6):
        for it in range(S // NT):
            t0 = it * NT
            pq = psq[it % 2]
            for c in range(2):
                mm(kb, pq[:, :], wkv[:, c, hd * 256:hd * 256 + 128], View(kvn, kvn.t[:, c, t0:t0 + NT]), c == 0, c == 1)
            kb.op("act", "copy", out=View(knT, knT.t[:, t0:t0 + NT]), in_=pq[:, :])
        for kbk in range(32):
            pq = psq[kbk % 2]
            for c in range(2):
                mm(kb, pq[:, 0:128], View(kvn, kvn.t[:, c, kbk * 128:(kbk + 1) * 128]),
                   wkv[:, c, hd * 256 + 128:hd * 256 + 256], c == 0, c == 1)
            kb.op("dve", "tensor_copy", out=vtm[:, kbk, :], in_=pq[:, 0:128])
        kb.dma("sp", qn[:, :], View(qn_d, qn_d.t[hd]))
        kb.dma("sp", qr[:, :], View(qr_d, qr_d.t[hd]))
        for c in range(S // NT):
            q0 = c * NT
            nk = 4 * c + 4
            for kbk in range(nk):
                sp = pss[ic % 2]
                pt = pts[ic % 2]
                ts = tss[ic % 2]
                ic += 1
                ks = slice(kbk * 128, (kbk + 1) * 128)
                mm(kb, sp[:, :], View(knT, knT.t[:, ks]), View(qn, qn.t[:, q0:q0 + NT]), True, False)
                mm(kb, sp[:, :], View(krT, krT.t[:, ks]), View(qr, qr.t[:, q0:q0 + NT]), False, True)
                if kbk >= 4 * c:
                    kb.op("dve", "scalar_tensor_tensor", out=ts[:, :], in0=sp[:, :], scalar=scale,
                          in1=masks[:, kbk - 4 * c, :], op0=ALU.mult, op1=ALU.add)
                    kb.op("act", "activation", out=pt[:, :], in_=ts[:, :], func=AF.Exp)
                else:
                    kb.op("act", "activation", out=pt[:, :], in_=sp[:, :], func=AF.Exp, scale=scale)
                mm(kb, pso[:, :], vtm[:, kbk, :], pt[:, :], kbk == 0, kbk == nk - 1)
                mm(kb, psd[:, :], onesb[:, :], pt[:, :], kbk == 0, kbk == nk - 1)
            kb.op("dve", "reciprocal", out=ysb[:, :], in_=psd[:, :])
            yo = ybf[c % 2]
            kb.op("dve", "tensor_tensor", out=yo[:, :], in0=ysb[:, :], in1=pso[:, :], op=ALU.mult)
            kb.dma("sp", View(mixT, mixT.t[1280 + hd * 128:1280 + (hd + 1) * 128, q0:q0 + NT]), yo[:, :])
    kb.barrier()
    es.close()


def esel_const():
    e = np.zeros((2, 64, 64, 2, 64), np.float32)
    for hp in range(2):
        for t in range(64):
            e[hp, t, t, hp, :] = 1.0
    return e.reshape(128, 64 * 128)


def blockones_const():
    b = np.zeros((128, 128), np.float32)
    b[0:64, 0:64] = 1.0
    b[64:128, 64:128] = 1.0
    return b


STRM = ("r", "ash", "wh", "wl", "b", "k")


def emit_rwkv_prep(kb, l, zT, wts, scr, cst):
    es = ExitStack()
    NT = 512
    bo = cst["blockones"]
    ident = cst["ident"]
    pr = cst["rwp"]
    mu = cst["rwmu"]
    wup = kb.sb([128, 768], BF16, "wup", es)
    kb.dma("pool", wup[0:64, :], View(wts["w_up"], wts["w_up"].t))
    kb.dma("pool", wup[64:128, :], View(wts["a_up"], wts["a_up"].t))
    gup = kb.sb([128, 768], BF16, "gup", es)
    kb.dma("pool", gup[:, :], View(wts["g_up"], wts["g_up"].t))
    Z = kb.sb([128, 20, NT + 1], F32, "rZ", es)
    ZS = kb.sb([128, 20, NT], F32, "rZS", es)
    lor = kb.sb([128, NT], BF16, "rlor", es)
    sg = kb.sb([128, NT], BF16, "rsg", es)
    TM = {s: kb.sb([128, 4, 768], BF16, "rTM" + s, es) for s in STRM}
    f = [kb.sb([128, NT], F32, "rf%d" % i, es) for i in range(10)]
    pw = kb.ps([128, NT], F32, "rpw", es)
    pa = kb.ps([128, NT], F32, "rpa", es)
    pg = kb.ps([128, NT], F32, "rpg", es)
    pss = kb.ps([128, NT], F32, "rpss", es)
    pbs = kb.ps([128, NT], F32, "rpbs", es)
    ptr = [kb.ps([128, 4, 128], F32, "rptr", es) for _ in range(2)]
    zv3 = zT.t[0:2560, :].rearrange("(c p) t -> p c t", p=128)
    kb.memset(TM["r"][:, :, :], 0.0)
    for s in STRM:
        kb.dma("sp", View(scr[s], scr[s].t[S:S + 64, :]), TM["r"][0:64, 0, :])
        kb.dma("sp", View(scr[s], scr[s].t[0:1, :]), TM["r"][0:1, 1, :])
    fm = lambda name: scr[name].t.rearrange("(c p) t -> p c t", p=128)
    itr = 0
    for it in range(S // NT):
        t0 = it * NT
        if it == 0:
            kb.memset(Z[:, :, 0:1], 0.0)
            kb.dma("sp", Z[:, :, 1:NT + 1], View(zT, zv3[:, :, 0:NT]))
        else:
            kb.dma("sp", Z[:, :, :], View(zT, zv3[:, :, t0 - 1:t0 + NT]))
        for c in range(20):
            e = "dve"
            kb.op(e, "tensor_tensor", out=ZS[:, c, :], in0=Z[:, c, 0:NT], in1=Z[:, c, 1:NT + 1], op=ALU.subtract)
            kb.op(e, "scalar_tensor_tensor", out=ZS[:, c, :], in0=ZS[:, c, :], scalar=mu[:, l * 20 + c:l * 20 + c + 1],
                  in1=Z[:, c, 1:NT + 1], op0=ALU.mult, op1=ALU.add)
        kb.op("act", "activation", out=lor[0:64, :], in_=ZS[0:64, 18, :], func=AF.Tanh)
        kb.op("act", "copy", out=lor[64:128, :], in_=ZS[64:128, 18, :])
        kb.op("act", "activation", out=sg[:, :], in_=ZS[:, 19, :], func=AF.Sigmoid)
        kb.dma("sp", View(scr["vsT"], fm("vsT")[:, :, t0:t0 + NT]), ZS[:, 12:18, :])
        for c in range(6):
            cs = slice(c * 128, (c + 1) * 128)
            P = lambda j: View(pr, pr.t[:, l, j, c:c + 1])
            mm(kb, pw[:, :], wup[0:64, cs], lor[0:64, :], True, True)
            mm(kb, pa[:, :], wup[64:128, cs], lor[64:128, :], True, True)
            mm(kb, pg[:, :], gup[:, cs], sg[:, :], True, True)
            e1, dec, a, kk, sq, kkn, t1, kp, bb, gsb = f
            kb.op("act", "activation", out=e1[:, :], in_=pw[:, :], func=AF.Exp, scale=-1.0, bias=P(7))
            kb.op("act", "activation", out=e1[:, :], in_=e1[:, :], func=AF.Ln, bias=1.0)
            kb.op("act", "activation", out=e1[:, :], in_=e1[:, :], func=AF.Exp, scale=-1.0, bias=-0.5)
            kb.op("act", "activation", out=dec[:, :], in_=e1[:, :], func=AF.Exp, scale=-1.0)
            kb.op("act", "activation", out=a[:, :], in_=pa[:, :], func=AF.Sigmoid, bias=P(1))
            kb.op("act", "copy", out=gsb[:, :], in_=pg[:, :])
            kb.dma("sp", View(scr["gT"], fm("gT")[:, c, t0:t0 + NT]), gsb[:, :])
            kb.op("dve", "tensor_scalar", out=kk[:, :], in0=ZS[:, 6 + c, :], scalar1=P(2), scalar2=None, op0=ALU.mult)
            kb.op("dve", "tensor_tensor", out=sq[:, :], in0=kk[:, :], in1=kk[:, :], op=ALU.mult)
            mm(kb, pss[:, :], bo[:, :], sq[:, :], True, True)
            rsqrt_eps(kb, sq[:, :], pss[:, :], 1e-24)
            kb.op("dve", "tensor_tensor", out=kkn[:, :], in0=kk[:, :], in1=sq[:, :], op=ALU.mult)
            kb.op("dve", "tensor_scalar", out=t1[:, :], in0=a[:, :], scalar1=-1.0, scalar2=P(3), op0=ALU.add, op1=ALU.mult)
            kb.op("dve", "scalar_tensor_tensor", out=kp[:, :], in0=t1[:, :], scalar=1.0, in1=ZS[:, 6 + c, :],
                  op0=ALU.add, op1=ALU.mult)
            kb.op("dve", "tensor_tensor", out=bb[:, :], in0=kkn[:, :], in1=a[:, :], op=ALU.mult)
            kb.op("dve", "tensor_scalar", out=kkn[:, :], in0=kkn[:, :], scalar1=-1.0, scalar2=None, op0=ALU.mult)
            kb.op("dve", "scalar_tensor_tensor", out=t1[:, :], in0=ZS[:, c, :], scalar=P(4), in1=kp[:, :],
                  op0=ALU.mult, op1=ALU.mult)
            mm(kb, pbs[:, :], bo[:, :], t1[:, :], True, True)
            kb.op("dve", "tensor_tensor", out=t1[:, :], in0=pbs[:, :], in1=ZS[:, 12 + c, :], op=ALU.mult)
            kb.dma("sp", View(scr["bonT"], fm("bonT")[:, c, t0:t0 + NT]), t1[:, :])
            for s, src in (("r", ZS[:, c, :]), ("ash", kkn[:, :]), ("wh", dec[:, :]), ("b", bb[:, :]), ("k", kp[:, :])):
                pt = ptr[itr % 2]
                itr += 1
                for sub in range(4):
                    kb.op("pe", "transpose", out=pt[:, sub, :], in_=View(src.buf, src.ap[:, sub * 128:(sub + 1) * 128]),
                          identity=ident[:, :])
                if s == "wh":
                    kb.op("act", "copy", out=TM["wh"][:, :, cs], in_=pt[:, :, :])
                    kb.op("dve", "tensor_tensor", out=TM["wl"][:, :, cs], in0=pt[:, :, :], in1=TM["wh"][:, :, cs],
                          op=ALU.subtract)
                else:
                    kb.op("act", "copy", out=TM[s][:, :, cs], in_=pt[:, :, :])
        for s in STRM:
            off = 0 if s == "ash" else 1
            kb.dma("sp", View(scr[s], scr[s].t[t0 + off:t0 + off + NT, :].rearrange("(s p) f -> p s f", p=128)), TM[s][:, :, :])
    kb.barrier()
    es.close()


def emit_rwkv_scan(kb, scr, cst):
    es = ExitStack()
    esel = kb.sb([128, 64, 128], BF16, "esel", es)
    kb.dma("pool", esel[:, :, :], View(cst["esel_d"], cst["esel_d"].t.rearrange("p (t q) -> p t q", q=128)))
    NH = 2
    St = [kb.sb([128, 3, 64], F32, "rS", es) for _ in range(NH)]
    T1 = [kb.sb([128, 3, 64], F32, "rT1", es) for _ in range(NH)]
    tmpa = [kb.sb([128, 3, 64], F32, "rtmpa", es) for _ in range(NH)]
    tmpr = [kb.sb([128, 3, 64], F32, "rtmpr", es) for _ in range(NH)]
    T2 = [[kb.sb([128, 3, 64], F32, "rT2", es) for _ in range(NH)] for _ in range(2)]
    Rsb = [kb.sb([128, 6, 64], F32, "rRsb", es) for _ in range(2)]
    SA = [[kb.sb([128, 64, 3], F32, "rSA", es) for _ in range(NH)] for _ in range(2)]
    YR = [[kb.sb([128, 64, 3], F32, "rYR", es) for _ in range(NH)] for _ in range(2)]
    for c in range(NH):
        kb.memset(St[c][:, :, :], 0.0)
        kb.memset(SA[1][c][:, :, :], 0.0)
    tiles = [{s: kb.sb([128, 6, 64], BF16, "rt" + s, es) for s in STRM} for _ in range(2)]
    vt = [kb.sb([128, 6, 64], F32, "rvt", es) for _ in range(2)]
    yb = [kb.sb([128, 6, 64], F32, "ryb", es) for _ in range(2)]
    pR = kb.ps([128, 512], F32, "pR", es)
    pA = kb.ps([128, 512], F32, "pA", es)
    pW = [kb.ps([128, 512], F32, "pW", es) for _ in range(2)]
    pB = [kb.ps([128, 512], F32, "pB", es) for _ in range(2)]
    pK = [kb.ps([128, 512], F32, "pK", es) for _ in range(2)]
    vv = scr["vsT"].t.rearrange("(c p) t -> p c t", p=128)
    yv = scr["yT"].t.rearrange("(c p) t -> p c t", p=128)
    f2 = lambda v: View(v.buf, v.ap.rearrange("p a b -> p (a b)"))
    h3 = lambda v: View(v.buf, v.ap.rearrange("p (a b) -> p a b", b=64))
    sa_prev = SA[1]
    sa_idx = 63
    st = 0
    for n in range(S // 64):
        t0 = n * 64
        tl = tiles[n % 2]
        for s in STRM:
            src = scr[s].t[t0 + 1:t0 + 65, :].rearrange("t (hf hp j) -> hp t hf j", hp=2, j=64)
            for hp in range(2):
                kb.dma("sp", tl[s][hp * 64:(hp + 1) * 64, :, :], View(scr[s], src[hp]))
        kb.dma("sp", vt[n % 2][:, :, :], View(scr["vsT"], vv[:, :, t0:t0 + 64]))
        sa_cur = SA[n % 2]
        yr_cur = YR[n % 2]
        for tp in range(64):
            E = esel[:, tp, :]
            w_, b_, k_ = pW[st % 2], pB[st % 2], pK[st % 2]
            t2 = T2[st % 2]
            rsb = Rsb[st % 2]
            st += 1
            mm(kb, k_[:, 0:384], E, f2(tl["k"][:, :, :]), True, True)
            mm(kb, w_[:, 0:384], E, f2(tl["wh"][:, :, :]), True, False)
            mm(kb, w_[:, 0:384], E, f2(tl["wl"][:, :, :]), False, True)
            mm(kb, b_[:, 0:384], E, f2(tl["b"][:, :, :]), True, True)
            mm(kb, pA[:, 0:384], E, f2(tl["ash"][:, :, :]), True, True)
            mm(kb, pR[:, 0:384], E, f2(tl["r"][:, :, :]), True, True)
            for hf in range(6):
                kb.op("act", "activation", out=t2[hf // 3][:, hf % 3, :], in_=k_[:, hf * 64:(hf + 1) * 64], func=AF.Copy,
                      scale=vt[n % 2][:, hf, tp:tp + 1])
            kb.op("act", "copy", out=f2(rsb[:, :, :]), in_=pR[:, 0:384])
            hs = [slice(c * 192, (c + 1) * 192) for c in range(NH)]
            for c in range(NH):
                kb.op("dve", "tensor_tensor", out=f2(St[c][:, :, :]), in0=f2(St[c][:, :, :]), in1=w_[:, hs[c]], op=ALU.mult)
            for c in range(NH):
                sab = View(sa_prev[c], sa_prev[c].t[:, sa_idx, :].unsqueeze(2).to_broadcast([128, 3, 64]))
                kb.op("dve", "tensor_tensor", out=T1[c][:, :, :], in0=h3(b_[:, hs[c]]), in1=sab, op=ALU.mult)
            for c in range(NH):
                kb.op("dve", "tensor_tensor", out=St[c][:, :, :], in0=St[c][:, :, :], in1=T1[c][:, :, :], op=ALU.add)
            for c in range(NH):
                kb.op("dve", "tensor_tensor", out=St[c][:, :, :], in0=St[c][:, :, :], in1=t2[c][:, :, :], op=ALU.add)
            for c in range(NH):
                kb.op("pool", "tensor_tensor", out=tmpr[c][:, :, :], in0=St[c][:, :, :], in1=rsb[:, 3 * c:3 * c + 3, :], op=ALU.mult)
            for c in range(NH):
                kb.op("dve", "tensor_tensor", out=tmpa[c][:, :, :], in0=h3(pA[:, hs[c]]), in1=St[c][:, :, :], op=ALU.mult)
            for c in range(NH):
                kb.op("dve", "tensor_reduce", out=sa_cur[c][:, tp, :], in_=tmpa[c][:, :, :], axis=AX.X, op=ALU.add)
            for c in range(NH):
                kb.op("dve", "tensor_reduce", out=yr_cur[c][:, tp, :], in_=tmpr[c][:, :, :], axis=AX.X, op=ALU.add)
            sa_prev, sa_idx = sa_cur, tp
        ybt = yb[n % 2]
        for c in range(NH):
            kb.op("act", "copy", out=ybt[:, 3 * c:3 * c + 3, :], in_=View(yr_cur[c], yr_cur[c].t.rearrange("p t c -> p c t")))
        kb.dma("sp", View(scr["yT"], yv[:, :, t0:t0 + 64]), ybt[:, :, :])
    kb.barrier()
    es.close()


def emit_rwkv_post(kb, l, scr, mixT, cst):
    es = ExitStack()
    NT = 512
    bo64 = cst["bo64"]
    pr = cst["rwp"]
    y = kb.sb([128, NT], F32, "py", es)
    g = kb.sb([128, NT], F32, "pgg", es)
    bn = kb.sb([128, NT], F32, "pbn", es)
    sq = kb.sb([128, NT], F32, "psq", es)
    ob = [kb.sb([128, NT], BF16, "pob", es) for _ in range(2)]
    pm = kb.ps([128, NT], F32, "ppm", es)
    pq = kb.ps([128, NT], F32, "ppq", es)
    fm = lambda name: scr[name].t.rearrange("(c p) t -> p c t", p=128)
    i = 0
    for it in range(S // NT):
        t0 = it * NT
        for c in range(6):
            P = lambda j: View(pr, pr.t[:, l, j, c:c + 1])
            kb.dma("sp", y[:, :], View(scr["yT"], fm("yT")[:, c, t0:t0 + NT]))
            kb.dma("sp", g[:, :], View(scr["gT"], fm("gT")[:, c, t0:t0 + NT]))
            kb.dma("sp", bn[:, :], View(scr["bonT"], fm("bonT")[:, c, t0:t0 + NT]))
            mm(kb, pm[:, :], bo64[:, :], y[:, :], True, True)
            kb.op("act", "activation", out=sq[:, :], in_=y[:, :], func=AF.Square)
            mm(kb, pq[:, :], bo64[:, :], sq[:, :], True, True)
            kb.op("dve", "tensor_tensor", out=y[:, :], in0=y[:, :], in1=pm[:, :], op=ALU.subtract)
            kb.op("act", "activation", out=sq[:, :], in_=pm[:, :], func=AF.Square)
            kb.op("dve", "tensor_tensor", out=sq[:, :], in0=pq[:, :], in1=sq[:, :], op=ALU.subtract)
            rsqrt_eps(kb, sq[:, :], sq[:, :], 64e-5)
            kb.op("dve", "tensor_tensor", out=y[:, :], in0=y[:, :], in1=sq[:, :], op=ALU.mult)
            kb.op("act", "activation", out=y[:, :], in_=y[:, :], func=AF.Identity, scale=P(5), bias=P(6))
            kb.op("dve", "tensor_tensor", out=y[:, :], in0=y[:, :], in1=bn[:, :], op=ALU.add)
            o = ob[i % 2]
            i += 1
            kb.op("dve", "tensor_tensor", out=o[:, :], in0=y[:, :], in1=g[:, :], op=ALU.mult)
            kb.dma("sp", View(mixT, mixT.t[c * 128:(c + 1) * 128, t0:t0 + NT]), o[:, :])
    kb.barrier()
    es.close()


def host_consts():
    c = {}
    c["c_onesf"] = np.full((128, 128), 1.0 / 2048.0, np.float32)
    c["c_ones512"] = np.full((128, 128), 1.0 / 512.0, np.float32)
    c["c_ones256"] = np.full((128, 128), 1.0 / 256.0, np.float32)
    c["c_ones1"] = np.full((128, 128), 1.0, np.float32)
    c["c_ident"] = np.eye(128, dtype=np.float32)
    c["c_blockones"] = blockones_const()
    c["c_bo64"] = blockones_const() / 64.0
    rs = np.zeros((32, 32, 128), np.float32)
    for e in range(32):
        rs[e, e, :] = 1.0
    c["c_rowsel"] = rs.reshape(32, 32 * 128)
    c["c_dbias"] = dil_consts()
    c["c_rope"] = rope_consts()
    c["c_mmask"] = mla_masks()
    c["c_esel"] = esel_const()
    return c


CONST_SHAPES = {"c_onesf": [128, 128], "c_ones512": [128, 128], "c_ones256": [128, 128], "c_ones1": [128, 128],
                "c_ident": [128, 128], "c_blockones": [128, 128], "c_bo64": [128, 128], "c_rowsel": [32, 4096],
                "c_dbias": [128, 3072], "c_rope": [64, 2 * S], "c_mmask": [128, 2048], "c_esel": [128, 8192]}


def build(phases="ABCDEFG", nlayers=DEPTH, test=False, ext_mix=False, ext_z=False):
    kb = KB()
    L = nlayers
    ext = lambda n, shp, dt=F32: kb.dram(n, shp, dt, "ExternalInput")
    scr = lambda n, shp, dt=F32: kb.dram(n, shp, dt, "ExternalOutput" if test else "Internal")
    x_in = ext("xT_in", [D, S])
    pT = ext("pT", [L, 256, S])
    w_in = ext("w_in", [L, D, IN_DIM])
    w_out = ext("w_out", [L, D, D])
    lnp = {n: ext(n, [128, L * 16]) for n in ("ln1_g", "ln1_b", "ln2_g", "ln2_b")}
    rw = ext("router_w", [D, 32]); rb = ext("router_b", [32])
    wg = ext("moe_w_gate", [L, 32, D, 512]); wu = ext("moe_w_up", [L, 32, D, 512]); wd = ext("moe_w_down", [L, 32, 512, D])
    wpp = ext("ple_w_proj", [L, 256, D]); wpg = ext("ple_w_gate", [L, D, D])
    rwp_d = ext("rwp", [128, L * 8 * 6]); rwmu_d = ext("rwmu", [128, L * 20])
    qag_d = ext("qag", [128, L * 4]); kvag_d = ext("kvag", [128, L * 2])
    w_up = ext("rwkv_w_up", [L, 64, 768]); a_up = ext("rwkv_a_up", [L, 64, 768]); g_up = ext("rwkv_g_up", [L, 128, 768])
    wuq = ext("mla_w_uq", [L, 512, 1152]); wukv = ext("mla_w_ukv", [L, 256, 1536])
    cd = {n: ext(n, shp) for n, shp in CONST_SHAPES.items()}
    outT = kb.dram("outT", [D, S], F32, "ExternalOutput")
    xT = scr("xT", [D, S])
    zT = ext("zT_in", [ZROWS, S]) if ext_z else scr("zT", [ZROWS, S])
    zv = ext("zv_in", [S, 1536]) if ext_z else scr("zv", [S, 1536])
    mixT = ext("mixT_in", [D, S], BF16) if ext_mix else scr("mixT", [D, S], BF16)
    x1T = scr("x1T", [D, S]); x1b = scr("x1b", [D, S], BF16); gates = scr("gatesT", [32, S]); ffnT = scr("ffnT", [D, S])
    rscr = {s: scr("strm_" + s, [S + 64, 768], BF16) for s in STRM}
    for n in ("vsT", "gT", "bonT", "yT"):
        rscr[n] = scr(n, [768, S])
    mscr = {"qn": scr("qn", [6, 128, S], BF16), "qr": scr("qr", [6, 64, S], BF16)}
    wsc = [[kb.dram("wsc_%d_%d" % (e, j), [128, 16, 512] if j < 2 else [128, 4, 2048], BF16, "Internal") for j in range(3)]
           for e in range(32)]

    es = ExitStack()
    cst = {"rowsel_d": cd["c_rowsel"], "dbias_d": cd["c_dbias"], "rope_d": cd["c_rope"], "mmask_d": cd["c_mmask"],
           "esel_d": cd["c_esel"]}

    def cload(name, src, shape, view=None, dt=F32, q="sp"):
        t = kb.sb(shape, dt, name, es)
        full = tuple(slice(None) for _ in shape)
        kb.dma(q, t[full], view if view is not None else View(src, src.t))
        cst[name] = t
    for n in ("onesf", "ones512", "ones256", "ident", "blockones", "bo64"):
        cload(n, cd["c_" + n], [128, 128])
    cload("onesb", cd["c_ones1"], [128, 128], dt=BF16, q="pool")
    cload("ln1g", lnp["ln1_g"], [128, L * 16]); cload("ln1b", lnp["ln1_b"], [128, L * 16])
    cload("ln2g", lnp["ln2_g"], [128, L * 16]); cload("ln2b", lnp["ln2_b"], [128, L * 16])
    cload("rw", rw, [128, 16, 32], View(rw, rw.t.rearrange("(c p) e -> p c e", p=128)))
    cload("rb", rb, [128, 32], View(rb, rb.t.partition_broadcast(128)))
    cload("rwp", rwp_d, [128, L, 8, 6], View(rwp_d, rwp_d.t.rearrange("p (l j c) -> p l j c", l=L, j=8)))
    cload("rwmu", rwmu_d, [128, L * 20])
    cload("qag", qag_d, [128, L * 4]); cload("kvag", kvag_d, [128, L * 2])
    rwp = cst["rwp"]
    for l in range(L):
        kb.op("dve", "tensor_scalar", out=rwp[:, l, 7, :], in0=rwp[:, l, 0, :], scalar1=-1.0, scalar2=None, op0=ALU.mult)
    xcur = x_in
    for l in range(L):
        xnext = outT if l == L - 1 else xT
        if "A" in phases:
            emit_inproj(kb, xcur, Buf(w_in.t[l], "w_in_l"), zT, zv)
        if "B" in phases:
            wts = {"w_up": Buf(w_up.t[l], "wup_l"), "a_up": Buf(a_up.t[l], "aup_l"), "g_up": Buf(g_up.t[l], "gup_l")}
            emit_rwkv_prep(kb, l, zT, wts, rscr, cst)
            emit_rwkv_scan(kb, rscr, cst)
            emit_rwkv_post(kb, l, rscr, mixT, cst)
        if "C" in phases:
            emit_dil(kb, zT, zv, mixT, cst)
        if "D" in phases:
            emit_mla(kb, l, zT, Buf(wuq.t[l], "wuq_l"), Buf(wukv.t[l], "wukv_l"), mixT, cst, mscr)
        if "E" in phases:
            emit_outproj(kb, l, mixT, Buf(w_out.t[l], "w_out_l"), xcur, x1T, x1b, gates, cst)
        if "F" in phases:
            emit_moe(kb, x1b, gates, Buf(wg.t[l], "wg_l"), Buf(wu.t[l], "wu_l"), Buf(wd.t[l], "wd_l"), ffnT, cst, wsc)
        if "G" in phases:
            emit_ple(kb, l, x1T, ffnT, Buf(pT.t[l], "pT_l"), Buf(wpg.t[l], "wpg_l"), Buf(wpp.t[l], "wpp_l"), xnext, cst)
        xcur = xnext
    es.close()
    return kb.finish()


def fm_cols(a, nch):
    L = a.shape[0]
    return np.ascontiguousarray(a.reshape(L, nch, 128).transpose(2, 0, 1).reshape(128, L * nch))


def host_inputs(inp, b, L=DEPTH):
    f = lambda k: np.asarray(inp[k], np.float32)
    im = dict(host_consts())
    im["xT_in"] = np.ascontiguousarray(f("x")[b].T)
    im["pT"] = np.ascontiguousarray(f("p")[:L, b].transpose(0, 2, 1))
    for k in ("w_in", "w_out", "moe_w_gate", "moe_w_up", "moe_w_down", "ple_w_proj", "ple_w_gate",
              "rwkv_w_up", "rwkv_a_up", "rwkv_g_up", "mla_w_uq", "mla_w_ukv"):
        im[k] = np.ascontiguousarray(f(k)[:L])
    im["router_w"] = f("router_w")
    im["router_b"] = f("router_b")
    for k in ("ln1_g", "ln1_b", "ln2_g", "ln2_b"):
        im[k] = fm_cols(f(k)[:L], 16)
    names = ("rwkv_w0", "rwkv_a0", "rwkv_k_k", "rwkv_k_a", "rwkv_r_k", "rwkv_gn_g", "rwkv_gn_b", "rwkv_w0")
    rwp = np.stack([f(n)[:L].reshape(L, 6, 128) for n in names], 1)
    im["rwp"] = np.ascontiguousarray(rwp.transpose(3, 0, 1, 2).reshape(128, L * 8 * 6))
    im["rwmu"] = fm_cols(f("rwkv_mu")[:L], 20)
    im["qag"] = fm_cols(f("mla_qa_g")[:L], 4)
    im["kvag"] = fm_cols(f("mla_kva_g")[:L], 2)
    return im


_PROG = {}


def kernel(**inputs):
    if "nc" not in _PROG:
        _PROG["nc"] = build()
    in_maps = [host_inputs(inputs, b) for b in range(B)]
    res = run_bass_kernel_spmd(_PROG["nc"], in_maps, core_ids=list(range(NCORES)))
    out = np.stack([np.asarray(res.results[b]["outT"], np.float32).T for b in range(B)], 0)
    return np.ascontiguousarray(out)
```

```python
import numpy as np
from contextlib import ExitStack
import concourse.bass as bass
import concourse.mybir as mybir
from concourse.bass_utils import run_bass_kernel_spmd

F32 = mybir.dt.float32
BF16 = mybir.dt.bfloat16
AF = mybir.ActivationFunctionType
ALU = mybir.AluOpType
AX = mybir.AxisListType

NCORES = 2
D = 2048
B = 2
S = 4096
DEPTH = 4
IN_DIM = 8000
ALPHA = (2 * DEPTH) ** 0.25
LN_EPS = 1e-5
RMS_EPS = 1e-6
NDS = 24


class View:
    __slots__ = ("buf", "ap")

    def __init__(self, buf, ap):
        self.buf = buf
        self.ap = ap

    def __getitem__(self, idx):
        return View(self.buf, self.ap[idx])


class Buf:
    def __init__(self, t, name):
        self.t = t
        self.name = name
        self.w = None
        self.r = {}

    def __getitem__(self, idx):
        return View(self, self.t[idx])

    def v(self, fn):
        return View(self, fn(self.t))


class KB:
    def __init__(self):
        self.nc = bass.Bass("TRN2", target_bir_lowering=False)
        self.es = ExitStack()
        nc = self.nc
        self.eng = {"pe": nc.tensor, "dve": nc.vector, "act": nc.scalar, "pool": nc.gpsimd, "sp": nc.sync}
        self.sem = {}
        self.cnt = {}
        for k in ("pe", "dve", "act", "pool"):
            self.sem[k] = self.es.enter_context(nc.semaphore("s_" + k))
            self.cnt[k] = 0
        self.seen = {k: {} for k in self.eng}
        self.dsem = [self.es.enter_context(nc.semaphore("d%d" % i)) for i in range(NDS)]
        self.dcnt = [0] * NDS
        self.dnext = 0
        self.out_events = []
        self.nuid = 0

    def uid(self, p):
        self.nuid += 1
        return "%s_%d" % (p, self.nuid)

    def sb(self, shape, dt, name="sb", es=None):
        nm = self.uid(name)
        t = (es or self.es).enter_context(self.nc.sbuf_tensor(nm, list(shape), dt))
        return Buf(t, nm)

    def ps(self, shape, dt=F32, name="ps", es=None):
        nm = self.uid(name)
        t = (es or self.es).enter_context(self.nc.psum_tensor(nm, list(shape), dt))
        return Buf(t, nm)

    def dram(self, name, shape, dt, kind):
        t = self.nc.dram_tensor(name, list(shape), dt, kind=kind).ap()
        return Buf(t, name)

    def _wait(self, e, ev):
        sem, val, src = ev
        if src == "pe" and e == "pe":
            return
        key = id(sem)
        if self.seen[e].get(key, 0) >= val:
            return
        self.seen[e][key] = val
        self.eng[e].wait_ge(sem, val)

    def _deps(self, e, reads, writes):
        for b in reads:
            if b.w is not None:
                self._wait(e, b.w)
        for b in writes:
            if b.w is not None:
                self._wait(e, b.w)
            for ev in b.r.values():
                self._wait(e, ev)

    def _commit(self, ev, reads, writes):
        for b in reads:
            if b not in writes:
                b.r[id(ev[0])] = ev
        for b in writes:
            b.w = ev
            b.r = {}

    def op(self, e, fn, *args, **kw):
        reads, writes = [], []
        kk = {}
        for k, v in kw.items():
            if isinstance(v, View):
                (writes if (k.startswith("out") or k == "accum_out") else reads).append(v.buf)
                kk[k] = v.ap
            else:
                kk[k] = v
        self._deps(e, reads, writes)
        ins = getattr(self.eng[e], fn)(*args, **kk)
        self.cnt[e] += 1
        ins.then_inc(self.sem[e], 1)
        ev = (self.sem[e], self.cnt[e], e)
        self._commit(ev, reads, writes)
        return ins

    def memset(self, view, val, e="dve"):
        self._deps(e, [], [view.buf])
        ins = self.eng[e].memset(view.ap, val)
        self.cnt[e] += 1
        ins.then_inc(self.sem[e], 1)
        self._commit((self.sem[e], self.cnt[e], e), [], [view.buf])

    def dma(self, q, out, in_, is_output=False):
        reads, writes = [], []
        if isinstance(out, View):
            writes.append(out.buf)
            out = out.ap
        if isinstance(in_, View):
            reads.append(in_.buf)
            in_ = in_.ap
        self._deps(q, reads, writes)
        i = self.dnext
        self.dnext = (self.dnext + 1) % NDS
        if self.dcnt[i] > 0:
            self._wait(q, (self.dsem[i], 16 * self.dcnt[i], "dma"))
        ins = self.eng[q].dma_start(out=out, in_=in_)
        self.dcnt[i] += 1
        ins.then_inc(self.dsem[i], 16)
        ev = (self.dsem[i], 16 * self.dcnt[i], "dma")
        self._commit(ev, reads, writes)
        if is_output:
            self.out_events.append(ev)
        return ins

    def barrier(self):
        for e in self.eng:
            for k in ("pe", "dve", "act", "pool"):
                if k != e and self.cnt[k] > 0:
                    self._wait(e, (self.sem[k], self.cnt[k], k))
            for i in range(NDS):
                if self.dcnt[i] > 0:
                    self._wait(e, (self.dsem[i], 16 * self.dcnt[i], "dma"))

    def finish(self):
        for ev in self.out_events:
            self._wait("sp", ev)
        self.es.close()
        return self.nc


def mm(kb, out, lhsT, rhs, start, stop):
    kb.op("pe", "matmul", out=out, lhsT=lhsT, rhs=rhs, start=start, stop=stop)


def mm(kb, out, lhsT, rhs, start, stop):
    kb.op("pe", "matmul", out=out, lhsT=lhsT, rhs=rhs, start=start, stop=stop)


def rsqrt_eps(kb, out, in_, eps):
    kb.op("act", "activation", out=out, in_=in_, func=AF.Ln, bias=float(eps))
    kb.op("act", "activation", out=out, in_=out, func=AF.Exp, scale=-0.5)


def dview(buf, pattern, **kw):
    return View(buf, buf.t.rearrange(pattern, **kw))


HOFF = 2560
DQ0, DK0, DV0 = 2560, 4096, 5632
QA0, KVA0, KR0 = 7168, 7680, 7936
ZROWS = 8064


def emit_inproj(kb, xT, w_in, zT, zv):
    es = ExitStack()
    NB = 512
    TH = 2048
    xbf = kb.sb([128, 16, TH], BF16, "xbf", es)
    wbufs = [kb.sb([128, 16, NB], BF16, "winb", es) for _ in range(2)]
    pss = [kb.ps([128, 512], F32, "pz", es) for _ in range(4)]
    pvs = [kb.ps([128, 512], F32, "pv", es) for _ in range(2)]
    zsb = [kb.sb([128, TH], F32, "zsb", es) for _ in range(2)]
    vsb = [kb.sb([128, 512], F32, "vsb", es) for _ in range(2)]
    wv = w_in.t.rearrange("(c p) n -> p c n", p=128)
    xv = xT.t.rearrange("(c p) t -> p c t", p=128)
    ib = im = ip = iv = 0
    for half in range(S // TH):
        h0 = half * TH
        for c0 in range(0, 16, 4):
            kb.dma("pool", xbf[:, c0:c0 + 4, :], View(xT, xv[:, c0:c0 + 4, h0:h0 + TH]))
        for nb in range(0, IN_DIM, NB):
            ncols = min(NB, IN_DIM - nb)
            wb = wbufs[ib % 2]
            ib += 1
            for c0 in range(0, 16, 8):
                kb.dma("pool", wb[:, c0:c0 + 8, 0:ncols], View(w_in, wv[:, c0:c0 + 8, nb:nb + ncols]))
            for m0 in range(0, ncols, 128):
                msz = min(128, ncols - m0)
                zs = zsb[im % 2]
                im += 1
                for th in range(TH // 512):
                    ps = pss[ip % 4]
                    ip += 1
                    for kc in range(16):
                        mm(kb, ps[0:msz, :], wb[:, kc, m0:m0 + msz], xbf[:, kc, th * 512:(th + 1) * 512],
                           kc == 0, kc == 15)
                    if th % 2 == 0:
                        kb.op("act", "copy", out=zs[0:msz, th * 512:(th + 1) * 512], in_=ps[0:msz, :])
                    else:
                        kb.op("dve", "tensor_copy", out=zs[0:msz, th * 512:(th + 1) * 512], in_=ps[0:msz, :])
                kb.dma("sp", View(zT, zT.t[nb + m0:nb + m0 + msz, h0:h0 + TH]), zs[0:msz, :])
            if DV0 <= nb < DV0 + 1536:
                for tt in range(TH // 128):
                    ps = pvs[iv % 2]
                    vs = vsb[iv % 2]
                    iv += 1
                    for kc in range(16):
                        mm(kb, ps[:, :], xbf[:, kc, tt * 128:(tt + 1) * 128], wb[:, kc, :], kc == 0, kc == 15)
                    if tt % 2 == 0:
                        kb.op("act", "copy", out=vs[:, :], in_=ps[:, :])
                    else:
                        kb.op("dve", "tensor_copy", out=vs[:, :], in_=ps[:, :])
                    kb.dma("sp", View(zv, zv.t[h0 + tt * 128:h0 + (tt + 1) * 128, nb - DV0:nb - DV0 + 512]), vs[:, :])
    kb.barrier()
    es.close()


def emit_ln(kb, h, NT, onesf, g, b, gi, pst, tmp, stat):
    pm, pq = pst
    for c in range(16):
        mm(kb, pm[:, :], onesf[:, :], h[:, c, :], c == 0, c == 15)
    for c in range(16):
        kb.op("act", "activation", out=tmp[:, :], in_=h[:, c, :], func=AF.Square)
        mm(kb, pq[:, :], onesf[:, :], tmp[:, :], c == 0, c == 15)
    mean = stat[:, 0, :]
    rstd = stat[:, 1, :]
    kb.op("act", "copy", out=mean, in_=pm[:, :])
    kb.op("dve", "tensor_tensor", out=tmp[:, :], in0=mean, in1=mean, op=ALU.mult)
    kb.op("dve", "tensor_tensor", out=rstd, in0=pq[:, :], in1=tmp[:, :], op=ALU.subtract)
    rsqrt_eps(kb, rstd, rstd, LN_EPS)
    for c in range(16):
        kb.op("dve", "tensor_tensor", out=h[:, c, :], in0=h[:, c, :], in1=mean, op=ALU.subtract)
        kb.op("dve", "tensor_tensor", out=h[:, c, :], in0=h[:, c, :], in1=rstd, op=ALU.mult)
        kb.op("act", "activation", out=h[:, c, :], in_=h[:, c, :], func=AF.Identity,
              scale=g[:, gi + c:gi + c + 1], bias=b[:, gi + c:gi + c + 1])


def emit_outproj(kb, l, mixT, w_out, xT, x1T, x1b, gates, cst):
    es = ExitStack()
    NT = 256
    wb = kb.sb([128, 16, 2048], BF16, "woutb", es)
    wv = w_out.t.rearrange("(c p) n -> p c n", p=128)
    for c0 in range(0, 16, 4):
        kb.dma("pool", wb[:, c0:c0 + 4, :], View(w_out, wv[:, c0:c0 + 4, :]))
    mxs = [kb.sb([128, 16, NT], BF16, "mixsb", es) for _ in range(2)]
    xs = kb.sb([128, 16, NT], F32, "xsb", es)
    h = kb.sb([128, 16, NT], F32, "hsb", es)
    hb = kb.sb([128, 16, NT], BF16, "hbsb", es)
    tmp = kb.sb([128, NT], F32, "lntmp", es)
    stat = kb.sb([128, 2, NT], F32, "lnstat", es)
    pso = [kb.ps([128, NT], F32, "po", es) for _ in range(3)]
    pst = [kb.ps([128, NT], F32, "pst", es) for _ in range(2)]
    prt = [kb.ps([128, 32], F32, "prt", es) for _ in range(2)]
    pgt = kb.ps([32, 128], F32, "pgt", es)
    gts = kb.sb([32, 128], F32, "gts", es)
    rs = [kb.sb([128, 32], F32, "rs%d" % i, es) for i in range(8)]
    r8 = [kb.sb([128, 8], F32, "r8%d" % i, es) for i in range(4)]
    r1 = [kb.sb([128, 1], F32, "r1%d" % i, es) for i in range(2)]
    mv = mixT.t.rearrange("(c p) t -> p c t", p=128)
    xv = xT.t.rearrange("(c p) t -> p c t", p=128)
    x1v = x1T.t.rearrange("(c p) t -> p c t", p=128)
    x1bv = x1b.t.rearrange("(c p) t -> p c t", p=128)
    for it in range(S // NT):
        t0 = it * NT
        mx = mxs[it % 2]
        kb.dma("sp", mx[:, :, :], View(mixT, mv[:, :, t0:t0 + NT]))
        kb.dma("sp", xs[:, :, :], View(xT, xv[:, :, t0:t0 + NT]))
        for oc in range(16):
            ps = pso[oc % 3]
            for kc in range(16):
                mm(kb, ps[:, :], wb[:, kc, oc * 128:(oc + 1) * 128], mx[:, kc, :], kc == 0, kc == 15)
            kb.op("dve", "scalar_tensor_tensor", out=h[:, oc, :], in0=xs[:, oc, :], scalar=ALPHA, in1=ps[:, :],
                  op0=ALU.mult, op1=ALU.add)
        emit_ln(kb, h, NT, cst["onesf"], cst["ln1g"], cst["ln1b"], l * 16, pst, tmp, stat)
        kb.dma("sp", View(x1T, x1v[:, :, t0:t0 + NT]), h[:, :, :])
        for c in range(16):
            if c % 2 == 0:
                kb.op("act", "copy", out=hb[:, c, :], in_=h[:, c, :])
            else:
                kb.op("pool", "tensor_copy", out=hb[:, c, :], in_=h[:, c, :])
        kb.dma("sp", View(x1b, x1bv[:, :, t0:t0 + NT]), hb[:, :, :])
        for sub in range(NT // 128):
            pr = prt[sub % 2]
            for kc in range(16):
                mm(kb, pr[:, :], h[:, kc, sub * 128:(sub + 1) * 128], cst["rw"][:, kc, :], kc == 0, kc == 15)
            sc, sel, eq1, sel2, eq2, gt, msk, gout = rs
            m1, m2, gs, gsel = r8
            gmax, den = r1
            kb.op("act", "activation", out=sc[:, :], in_=pr[:, :], func=AF.Sigmoid)
            kb.op("dve", "tensor_tensor", out=sel[:, :], in0=sc[:, :], in1=cst["rb"][:, :], op=ALU.add)
            g3 = lambda v: View(v.buf, v.ap.rearrange("p (g e) -> p g e", e=4))
            b3 = lambda v: View(v.buf, v.ap.unsqueeze(2).to_broadcast([128, 8, 4]))
            kb.op("dve", "tensor_reduce", out=m1[:, :], in_=g3(sel[:, :]), axis=AX.X, op=ALU.max)
            kb.op("dve", "tensor_tensor", out=g3(eq1[:, :]), in0=g3(sel[:, :]), in1=b3(m1[:, :]), op=ALU.is_equal)
            kb.op("dve", "scalar_tensor_tensor", out=sel2[:, :], in0=eq1[:, :], scalar=-1.0e4, in1=sel[:, :],
                  op0=ALU.mult, op1=ALU.add)
            kb.op("dve", "tensor_reduce", out=m2[:, :], in_=g3(sel2[:, :]), axis=AX.X, op=ALU.max)
            kb.op("dve", "tensor_tensor", out=g3(eq2[:, :]), in0=g3(sel2[:, :]), in1=b3(m2[:, :]), op=ALU.is_equal)
            kb.op("dve", "tensor_tensor", out=gs[:, :], in0=m1[:, :], in1=m2[:, :], op=ALU.add)
            kb.op("dve", "tensor_reduce", out=gmax[:, :], in_=gs[:, :], axis=AX.X, op=ALU.max)
            kb.op("dve", "tensor_scalar", out=gsel[:, :], in0=gs[:, :], scalar1=gmax[:, 0:1], scalar2=None,
                  op0=ALU.is_equal)
            kb.op("dve", "tensor_tensor", out=msk[:, :], in0=eq1[:, :], in1=eq2[:, :], op=ALU.add)
            kb.op("dve", "tensor_tensor", out=g3(msk[:, :]), in0=g3(msk[:, :]), in1=b3(gsel[:, :]), op=ALU.mult)
            kb.op("dve", "tensor_tensor", out=gt[:, :], in0=msk[:, :], in1=sc[:, :], op=ALU.mult)
            kb.op("dve", "tensor_reduce", out=den[:, :], in_=gt[:, :], axis=AX.X, op=ALU.add)
            kb.op("dve", "reciprocal", out=den[:, :], in_=den[:, :])
            kb.op("dve", "tensor_scalar", out=gout[:, :], in0=gt[:, :], scalar1=den[:, 0:1], scalar2=None,
                  op0=ALU.mult)
            kb.op("pe", "transpose", out=pgt[:, :], in_=gout[:, :], identity=cst["ident"][:, :])
            kb.op("act", "copy", out=gts[:, :], in_=pgt[:, :])
            kb.dma("sp", View(gates, gates.t[:, t0 + sub * 128:t0 + (sub + 1) * 128]), gts[:, :])
    kb.barrier()
    es.close()


def emit_moe(kb, x1b, gates, wg, wu, wd, ffnT, cst, wsc):
    es = ExitStack()
    NT = 512
    xs = kb.sb([128, 16, NT], BF16, "mx", es)
    grow = kb.sb([32, NT], F32, "grow", es)
    rowsel = kb.sb([32, 32, 128], F32, "rowsel", es)
    kb.dma("sp", rowsel[:, :, :], View(cst["rowsel_d"], cst["rowsel_d"].t.rearrange("k (e p) -> k e p", p=128)))
    y = kb.sb([128, 16, NT], F32, "my", es)
    wgb = [kb.sb([128, 16, 512], BF16, "wgb", es) for _ in range(2)]
    wub = [kb.sb([128, 16, 512], BF16, "wub", es) for _ in range(2)]
    wdb = [kb.sb([128, 4, 2048], BF16, "wdb", es) for _ in range(2)]
    hg = [kb.sb([128, 4, NT], BF16, "hg", es) for _ in range(2)]
    sg = [kb.sb([128, NT], F32, "sg", es) for _ in range(2)]
    pg = [kb.ps([128, NT], F32, "pg", es) for _ in range(2)]
    pu = [kb.ps([128, NT], F32, "pu", es) for _ in range(2)]
    pd = [kb.ps([128, NT], F32, "pd", es) for _ in range(3)]
    pb = kb.ps([128, NT], F32, "pb", es)
    xv = x1b.t.rearrange("(c p) t -> p c t", p=128)
    fv = ffnT.t.rearrange("(c p) t -> p c t", p=128)
    ie = 0
    for it in range(S // NT):
        t0 = it * NT
        kb.dma("sp", xs[:, :, :], View(x1b, xv[:, :, t0:t0 + NT]))
        kb.dma("sp", grow[:, :], View(gates, gates.t[:, t0:t0 + NT]))
        for e in range(32):
            b = ie % 2
            ie += 1
            if it == 0:
                kb.dma("pool", wgb[b][:, :, :], View(wg, wg.t[e].rearrange("(c p) n -> p c n", p=128)))
                kb.dma("pool", wub[b][:, :, :], View(wu, wu.t[e].rearrange("(c p) n -> p c n", p=128)))
                kb.dma("pool", wdb[b][:, :, :], View(wd, wd.t[e].rearrange("(c p) n -> p c n", p=128)))
                kb.dma("sp", View(wsc[e][0], wsc[e][0].t), wgb[b][:, :, :])
                kb.dma("sp", View(wsc[e][1], wsc[e][1].t), wub[b][:, :, :])
                kb.dma("sp", View(wsc[e][2], wsc[e][2].t), wdb[b][:, :, :])
            else:
                kb.dma("sp", wgb[b][:, :, :], View(wsc[e][0], wsc[e][0].t))
                kb.dma("act", wub[b][:, :, :], View(wsc[e][1], wsc[e][1].t))
                kb.dma("sp", wdb[b][:, :, :], View(wsc[e][2], wsc[e][2].t))
            hgt = hg[b]
            mm(kb, pb[:, :], rowsel[0:32, e, :], grow[0:32, :], True, True)
            for hc in range(4):
                p1 = pg[hc % 2]
                p2 = pu[hc % 2]
                s1 = sg[hc % 2]
                for kc in range(16):
                    mm(kb, p1[:, :], wgb[b][:, kc, hc * 128:(hc + 1) * 128], xs[:, kc, :], kc == 0, kc == 15)
                for kc in range(16):
                    mm(kb, p2[:, :], wub[b][:, kc, hc * 128:(hc + 1) * 128], xs[:, kc, :], kc == 0, kc == 15)
                kb.op("act", "activation", out=s1[:, :], in_=p1[:, :], func=AF.Silu)
                kb.op("dve", "tensor_tensor", out=s1[:, :], in0=s1[:, :], in1=p2[:, :], op=ALU.mult)
                kb.op("dve", "tensor_tensor", out=hgt[:, hc, :], in0=s1[:, :], in1=pb[:, :], op=ALU.mult)
            for (ed, bd) in ([(e - 1, 1 - b)] if e > 0 else []) + ([(e, b)] if e == 31 else []):
                for oc in range(16):
                    ps = pd[oc % 3]
                    for hc in range(4):
                        mm(kb, ps[:, :], wdb[bd][:, hc, oc * 128:(oc + 1) * 128], hg[bd][:, hc, :], hc == 0, hc == 3)
                    if ed == 0:
                        kb.op("act", "copy", out=y[:, oc, :], in_=ps[:, :])
                    else:
                        kb.op("dve", "tensor_tensor", out=y[:, oc, :], in0=y[:, oc, :], in1=ps[:, :], op=ALU.add)
        kb.dma("sp", View(ffnT, fv[:, :, t0:t0 + NT]), y[:, :, :])
    kb.barrier()
    es.close()


def emit_ple(kb, l, x1T, ffnT, pT, wpg, wpp, xoT, cst):
    es = ExitStack()
    NT = 256
    wb = kb.sb([128, 16, 2048], BF16, "wpgb", es)
    wv = wpg.t.rearrange("(c p) n -> p c n", p=128)
    for c0 in range(0, 16, 4):
        kb.dma("pool", wb[:, c0:c0 + 4, :], View(wpg, wv[:, c0:c0 + 4, :]))
    wpb = kb.sb([128, 2, 2048], BF16, "wppb", es)
    kb.dma("pool", wpb[:, :, :], View(wpp, wpp.t.rearrange("(c p) n -> p c n", p=128)))
    h = kb.sb([128, 16, NT], F32, "gh", es)
    f = kb.sb([128, 16, NT], F32, "gf", es)
    hb = kb.sb([128, 16, NT], BF16, "ghb", es)
    pb = kb.sb([128, 2, NT], BF16, "gpb", es)
    tmp = kb.sb([128, NT], F32, "gtmp", es)
    stat = kb.sb([128, 2, NT], F32, "gstat", es)
    sgs = [kb.sb([128, NT], F32, "gsg", es) for _ in range(2)]
    pst = [kb.ps([128, NT], F32, "gpst", es) for _ in range(2)]
    pgs = [kb.ps([128, NT], F32, "gpg", es) for _ in range(2)]
    pps = [kb.ps([128, NT], F32, "gpp", es) for _ in range(2)]
    x1v = x1T.t.rearrange("(c p) t -> p c t", p=128)
    fv = ffnT.t.rearrange("(c p) t -> p c t", p=128)
    ov = xoT.t.rearrange("(c p) t -> p c t", p=128)
    pv = pT.t.rearrange("(c p) t -> p c t", p=128)
    for it in range(S // NT):
        t0 = it * NT
        kb.dma("sp", h[:, :, :], View(x1T, x1v[:, :, t0:t0 + NT]))
        kb.dma("sp", f[:, :, :], View(ffnT, fv[:, :, t0:t0 + NT]))
        kb.dma("pool", pb[:, :, :], View(pT, pv[:, :, t0:t0 + NT]))
        for c in range(16):
            kb.op("dve", "scalar_tensor_tensor", out=h[:, c, :], in0=h[:, c, :], scalar=ALPHA, in1=f[:, c, :],
                  op0=ALU.mult, op1=ALU.add)
        emit_ln(kb, h, NT, cst["onesf"], cst["ln2g"], cst["ln2b"], l * 16, pst, tmp, stat)
        for c in range(16):
            if c % 2 == 0:
                kb.op("act", "copy", out=hb[:, c, :], in_=h[:, c, :])
            else:
                kb.op("pool", "tensor_copy", out=hb[:, c, :], in_=h[:, c, :])
        for oc in range(16):
            p1 = pgs[oc % 2]
            p2 = pps[oc % 2]
            s1 = sgs[oc % 2]
            for kc in range(16):
                mm(kb, p1[:, :], wb[:, kc, oc * 128:(oc + 1) * 128], hb[:, kc, :], kc == 0, kc == 15)
            for kc in range(2):
                mm(kb, p2[:, :], wpb[:, kc, oc * 128:(oc + 1) * 128], pb[:, kc, :], kc == 0, kc == 1)
            kb.op("act", "activation", out=s1[:, :], in_=p1[:, :], func=AF.Sigmoid)
            kb.op("dve", "tensor_tensor", out=s1[:, :], in0=s1[:, :], in1=p2[:, :], op=ALU.mult)
            kb.op("dve", "tensor_tensor", out=f[:, oc, :], in0=h[:, oc, :], in1=s1[:, :], op=ALU.add)
        kb.dma("sp", View(xoT, ov[:, :, t0:t0 + NT]), f[:, :, :], is_output=True)
    kb.barrier()
    es.close()


DIL = (1, 4, 16)


def dil_consts():
    out = np.zeros((128, 4, 3, 2, 128), np.float32)
    k = np.arange(128)[:, None]
    q = np.arange(128)[None, :]
    for slot in range(4):
        for g, d in enumerate(DIL):
            head = g * 4 + slot
            slope = 2.0 ** (-8.0 * (head + 1) / 12.0)
            out[:, slot, g, 0, :] = np.where(k >= q, -slope * d * (q - k + 128), -30000.0)
            out[:, slot, g, 1, :] = np.where(k <= q, -slope * d * (q - k), -30000.0)
    return out.reshape(128, 4 * 3 * 2 * 128)


def emit_dil(kb, zT, zv, mixT, cst):
    es = ExitStack()
    bias = kb.sb([128, 4, 3, 2, 128], F32, "dbias", es)
    kb.dma("sp", bias[:, :, :, :, :], View(cst["dbias_d"], cst["dbias_d"].t.rearrange("p (s g h q) -> p s g h q", s=4, g=3, h=2)))
    onesb = cst["onesb"]
    qT = kb.sb([128, S], BF16, "dqT", es)
    kT = kb.sb([128, S], BF16, "dkT", es)
    vb = [kb.sb([128, 128], BF16, "dvb", es) for _ in range(3)]
    num = kb.sb([128, S], F32, "dnum", es)
    den = kb.sb([128, S], F32, "dden", es)
    yb = kb.sb([128, S], BF16, "dyb", es)
    sps = [kb.ps([128, 256], F32, "dsp", es) for _ in range(2)]
    ops = [kb.ps([128, 128], F32, "dop", es) for _ in range(2)]
    dps = [kb.ps([128, 128], F32, "ddp", es) for _ in range(2)]
    tsb = [kb.sb([128, 256], F32, "dts", es) for _ in range(2)]
    pT = [kb.sb([128, 256], BF16, "dpT", es) for _ in range(2)]
    scale = 128.0 ** -0.5
    for slot in range(4):
        for g, d in enumerate(DIL):
            head = g * 4 + slot
            nbs = 32 // d
            kb.dma("pool", qT[:, :], View(zT, zT.t[DQ0 + head * 128:DQ0 + (head + 1) * 128, :]))
            kb.dma("pool", kT[:, :], View(zT, zT.t[DK0 + head * 128:DK0 + (head + 1) * 128, :]))
            for n in range(32):
                r, m = divmod(n, nbs)
                has_prev = m > 0
                t0 = r + d * 128 * m
                tok = slice(t0, t0 + d * 127 + 1, d)
                tokp = slice(t0 - d * 128, t0 - d * 128 + d * 127 + 1, d)
                vcur = vb[n % 3]
                kb.dma("pool", vcur[:, :], View(zv, zv.t[tok, head * 128:(head + 1) * 128]))
                vprev = vb[(n - 1) % 3]
                sp = sps[n % 2]
                ts = tsb[n % 2]
                pt = pT[n % 2]
                op_ = ops[n % 2]
                dp_ = dps[n % 2]
                lo = 0 if has_prev else 1
                if has_prev:
                    mm(kb, sp[:, 0:128], View(kT, kT.t[:, tokp]), View(qT, qT.t[:, tok]), True, True)
                mm(kb, sp[:, 128:256], View(kT, kT.t[:, tok]), View(qT, qT.t[:, tok]), True, True)
                bv = View(bias, bias.t[:, slot, g, lo:2, :].rearrange("p h q -> p (h q)"))
                kb.op("dve", "scalar_tensor_tensor", out=ts[:, lo * 128:256], in0=sp[:, lo * 128:256], scalar=scale,
                      in1=bv, op0=ALU.mult, op1=ALU.add)
                kb.op("act", "activation", out=pt[:, lo * 128:256], in_=ts[:, lo * 128:256], func=AF.Exp)
                if has_prev:
                    mm(kb, op_[:, :], vprev[:, :], pt[:, 0:128], True, False)
                mm(kb, op_[:, :], vcur[:, :], pt[:, 128:256], not has_prev, True)
                if has_prev:
                    mm(kb, dp_[:, :], onesb[:, :], pt[:, 0:128], True, False)
                mm(kb, dp_[:, :], onesb[:, :], pt[:, 128:256], not has_prev, True)
                nv = View(num, num.t[:, tok])
                dv = View(den, den.t[:, tok])
                if g == 0:
                    kb.op("act", "copy", out=nv, in_=op_[:, :])
                    kb.op("dve", "tensor_copy", out=dv, in_=dp_[:, :])
                else:
                    kb.op("dve", "tensor_tensor", out=nv, in0=nv, in1=op_[:, :], op=ALU.add)
                    kb.op("dve", "tensor_tensor", out=dv, in0=dv, in1=dp_[:, :], op=ALU.add)
        kb.op("dve", "reciprocal", out=den[:, :], in_=den[:, :])
        kb.op("dve", "tensor_tensor", out=yb[:, :], in0=num[:, :], in1=den[:, :], op=ALU.mult)
        kb.dma("sp", View(mixT, mixT.t[768 + slot * 128:768 + (slot + 1) * 128, :]), yb[:, :])
    kb.barrier()
    es.close()


def rope_consts():
    half = 32
    inv = 10000.0 ** (-np.arange(half, dtype=np.float32) / half)
    ang = np.arange(S, dtype=np.float32)[None, :] * inv[:, None]
    cos = np.concatenate([np.cos(ang), np.cos(ang)], 0)
    sin = np.concatenate([-np.sin(ang), np.sin(ang)], 0)
    return np.stack([cos, sin], 1).astype(np.float32).reshape(64, 2 * S)


def mla_masks():
    k = np.arange(128)[:, None, None] + 128 * np.arange(4)[None, :, None]
    q = np.arange(512)[None, None, :]
    return np.where(k <= q, 0.0, -30000.0).astype(np.float32).reshape(128, 4 * 512)


def emit_rms(kb, zt, nch, NT, gam, ones_sc, ps, tmp, out_bf):
    for c in range(nch):
        kb.op("act", "activation", out=tmp[:, :], in_=zt[:, c, :], func=AF.Square)
        mm(kb, ps[:, :], ones_sc[:, :], tmp[:, :], c == 0, c == nch - 1)
    rsqrt_eps(kb, tmp[:, :], ps[:, :], RMS_EPS)
    for c in range(nch):
        kb.op("dve", "scalar_tensor_tensor", out=out_bf[:, c, :], in0=zt[:, c, :], scalar=gam[:, c:c + 1], in1=tmp[:, :],
              op0=ALU.mult, op1=ALU.mult)


def emit_mla(kb, l, zT, wuq, wukv, mixT, cst, scr):
    es = ExitStack()
    NT = 512
    masks = kb.sb([128, 4, 512], F32, "mmask", es)
    kb.dma("sp", masks[:, :, :], View(cst["mmask_d"], cst["mmask_d"].t.rearrange("p (a q) -> p a q", a=4)))
    onesb = cst["onesb"]
    wkv = kb.sb([128, 2, 1536], BF16, "wkv", es)
    kb.dma("pool", wkv[:, :, :], View(wukv, wukv.t.rearrange("(c p) n -> p c n", p=128)))
    kvn = kb.sb([128, 2, S], BF16, "kvn", es)
    krT = kb.sb([64, S], BF16, "krT", es)
    psq = [kb.ps([128, NT], F32, "mpsq", es) for _ in range(2)]
    es1 = ExitStack()
    rope = kb.sb([64, 2, S], F32, "rope", es1)
    kb.dma("sp", rope[:, :, :], View(cst["rope_d"], cst["rope_d"].t.rearrange("p (a t) -> p a t", a=2)))
    wq = kb.sb([128, 4, 1152], BF16, "wq", es1)
    kb.dma("pool", wq[:, :, :], View(wuq, wuq.t.rearrange("(c p) n -> p c n", p=128)))
    wqs = kb.sb([128, 4, 6, 64], BF16, "wqs", es1)
    wq4 = wuq.t.rearrange("(c p) (h e) -> p c h e", p=128, e=192)
    for c in range(4):
        kb.dma("pool", wqs[:, c, :, 0:32], View(wuq, wq4[:, c, :, 160:192]))
        kb.dma("pool", wqs[:, c, :, 32:64], View(wuq, wq4[:, c, :, 128:160]))
    zt = kb.sb([128, 4, NT], F32, "mzt", es1)
    zb = kb.sb([128, 4, NT], BF16, "mzb", es1)
    tmp = kb.sb([128, NT], F32, "mtmp", es1)
    t64 = [kb.sb([64, NT], F32, "mt64", es1) for _ in range(3)]
    qst = [kb.sb([128, NT], BF16, "mqst", es1) for _ in range(2)]
    qrs = [kb.sb([64, NT], BF16, "mqrs", es1) for _ in range(2)]
    ps1 = kb.ps([128, NT], F32, "mps1", es1)
    psr = [kb.ps([64, NT], F32, "mpsr", es1) for _ in range(2)]
    qn_d, qr_d = scr["qn"], scr["qr"]
    for it in range(S // NT):
        t0 = it * NT
        kb.dma("sp", zt[:, :, :], View(zT, zT.t[QA0:QA0 + 512, t0:t0 + NT].rearrange("(c p) t -> p c t", p=128)))
        emit_rms(kb, zt, 4, NT, View(cst["qag"], cst["qag"].t[:, l * 4:(l + 1) * 4]), cst["ones512"], ps1, tmp, zb)
        for hd in range(6):
            pq = psq[hd % 2]
            for c in range(4):
                mm(kb, pq[:, :], wq[:, c, hd * 192:hd * 192 + 128], zb[:, c, :], c == 0, c == 3)
            qs = qst[hd % 2]
            kb.op("act", "copy", out=qs[:, :], in_=pq[:, :])
            kb.dma("sp", View(qn_d, qn_d.t[hd, :, t0:t0 + NT]), qs[:, :])
            pr, pw = psr
            for c in range(4):
                mm(kb, pr[:, :], wq[:, c, hd * 192 + 128:hd * 192 + 192], zb[:, c, :], c == 0, c == 3)
            for c in range(4):
                mm(kb, pw[:, :], wqs[:, c, hd, :], zb[:, c, :], c == 0, c == 3)
            a, b2, _ = t64
            kb.op("dve", "tensor_tensor", out=a[:, :], in0=pr[:, :], in1=View(rope, rope.t[:, 0, t0:t0 + NT]), op=ALU.mult)
            kb.op("dve", "tensor_tensor", out=b2[:, :], in0=pw[:, :], in1=View(rope, rope.t[:, 1, t0:t0 + NT]), op=ALU.mult)
            qr_ = qrs[hd % 2]
            kb.op("dve", "tensor_tensor", out=qr_[:, :], in0=a[:, :], in1=b2[:, :], op=ALU.add)
            kb.dma("sp", View(qr_d, qr_d.t[hd, :, t0:t0 + NT]), qr_[:, :])
        kb.dma("sp", zt[:, 0:2, :], View(zT, zT.t[KVA0:KVA0 + 256, t0:t0 + NT].rearrange("(c p) t -> p c t", p=128)))
        emit_rms(kb, zt, 2, NT, View(cst["kvag"], cst["kvag"].t[:, l * 2:(l + 1) * 2]), cst["ones256"], ps1, tmp,
                 View(kvn, kvn.t[:, :, t0:t0 + NT]))
        a, b2, c2 = t64
        kb.dma("sp", a[:, :], View(zT, zT.t[KR0:KR0 + 64, t0:t0 + NT]))
        kb.dma("sp", b2[0:32, :], View(zT, zT.t[KR0 + 32:KR0 + 64, t0:t0 + NT]))
        kb.dma("sp", b2[32:64, :], View(zT, zT.t[KR0:KR0 + 32, t0:t0 + NT]))
        kb.op("dve", "tensor_tensor", out=a[:, :], in0=a[:, :], in1=View(rope, rope.t[:, 0, t0:t0 + NT]), op=ALU.mult)
        kb.op("dve", "tensor_tensor", out=b2[:, :], in0=b2[:, :], in1=View(rope, rope.t[:, 1, t0:t0 + NT]), op=ALU.mult)
        kb.op("dve", "tensor_tensor", out=View(krT, krT.t[:, t0:t0 + NT]), in0=a[:, :], in1=b2[:, :], op=ALU.add)
    kb.barrier()
    es1.close()
    knT = kb.sb([128, S], BF16, "knT", es)
    vtm = kb.sb([128, 32, 128], BF16, "vtm", es)
    qn = kb.sb([128, S], BF16, "qnT", es)
    qr = kb.sb([64, S], BF16, "qrT", es)
    pts = [kb.sb([128, NT], BF16, "mpt", es) for _ in range(2)]
    tss = [kb.sb([128, NT], F32, "mts", es) for _ in range(2)]
    ysb = kb.sb([128, NT], F32, "mys", es)
    ybf = [kb.sb([128, NT], BF16, "mybf", es) for _ in range(2)]
    pss = [kb.ps([128, NT], F32, "mpss", es) for _ in range(2)]
    pso = kb.ps([128, NT], F32, "mpso", es)
    psd = kb.ps([128, NT], F32, "mpsd", es)
    scale = 192.0 ** -0.5
    ic = 0
    for hd in range(6):
        for it in range(S // NT):
            t0 = it * NT
            pq = psq[it % 2]
            for c in range(2):
                mm(kb, pq[:, :], wkv[:, c, hd * 256:hd * 256 + 128], View(kvn, kvn.t[:, c, t0:t0 + NT]), c == 0, c == 1)
            kb.op("act", "copy", out=View(knT, knT.t[:, t0:t0 + NT]), in_=pq[:, :])
        for kbk in range(32):
            pq = psq[kbk % 2]
            for c in range(2):
                mm(kb, pq[:, 0:128], View(kvn, kvn.t[:, c, kbk * 128:(kbk + 1) * 128]),
                   wkv[:, c, hd * 256 + 128:hd * 256 + 256], c == 0, c == 1)
            kb.op("dve", "tensor_copy", out=vtm[:, kbk, :], in_=pq[:, 0:128])
        kb.dma("sp", qn[:, :], View(qn_d, qn_d.t[hd]))
        kb.dma("sp", qr[:, :], View(qr_d, qr_d.t[hd]))
        for c in range(S // NT):
            q0 = c * NT
            nk = 4 * c + 4
            for kbk in range(nk):
                sp = pss[ic % 2]
                pt = pts[ic % 2]
                ts = tss[ic % 2]
                ic += 1
                ks = slice(kbk * 128, (kbk + 1) * 128)
                mm(kb, sp[:, :], View(knT, knT.t[:, ks]), View(qn, qn.t[:, q0:q0 + NT]), True, False)
                mm(kb, sp[:, :], View(krT, krT.t[:, ks]), View(qr, qr.t[:, q0:q0 + NT]), False, True)
                if kbk >= 4 * c:
                    kb.op("dve", "scalar_tensor_tensor", out=ts[:, :], in0=sp[:, :], scalar=scale,
                          in1=masks[:, kbk - 4 * c, :], op0=ALU.mult, op1=ALU.add)
                    kb.op("act", "activation", out=pt[:, :], in_=ts[:, :], func=AF.Exp)
                else:
                    kb.op("act", "activation", out=pt[:, :], in_=sp[:, :], func=AF.Exp, scale=scale)
                mm(kb, pso[:, :], vtm[:, kbk, :], pt[:, :], kbk == 0, kbk == nk - 1)
                mm(kb, psd[:, :], onesb[:, :], pt[:, :], kbk == 0, kbk == nk - 1)
            kb.op("dve", "reciprocal", out=ysb[:, :], in_=psd[:, :])
            yo = ybf[c % 2]
            kb.op("dve", "tensor_tensor", out=yo[:, :], in0=ysb[:, :], in1=pso[:, :], op=ALU.mult)
            kb.dma("sp", View(mixT, mixT.t[1280 + hd * 128:1280 + (hd + 1) * 128, q0:q0 + NT]), yo[:, :])
    kb.barrier()
    es.close()


def esel_const():
    e = np.zeros((2, 64, 64, 2, 64), np.float32)
    for hp in range(2):
        for t in range(64):
            e[hp, t, t, hp, :] = 1.0
    return e.reshape(128, 64 * 128)


def blockones_const():
    b = np.zeros((128, 128), np.float32)
    b[0:64, 0:64] = 1.0
    b[64:128, 64:128] = 1.0
    return b


STRM = ("r", "ash", "wh", "wl", "b", "k")


def emit_rwkv_prep(kb, l, zT, wts, scr, cst):
    es = ExitStack()
    NT = 512
    bo = cst["blockones"]
    ident = cst["ident"]
    pr = cst["rwp"]
    mu = cst["rwmu"]
    wup = kb.sb([128, 768], BF16, "wup", es)
    kb.dma("pool", wup[0:64, :], View(wts["w_up"], wts["w_up"].t))
    kb.dma("pool", wup[64:128, :], View(wts["a_up"], wts["a_up"].t))
    gup = kb.sb([128, 768], BF16, "gup", es)
    kb.dma("pool", gup[:, :], View(wts["g_up"], wts["g_up"].t))
    Z = kb.sb([128, 20, NT + 1], F32, "rZ", es)
    ZS = kb.sb([128, 20, NT], F32, "rZS", es)
    lor = kb.sb([128, NT], BF16, "rlor", es)
    sg = kb.sb([128, NT], BF16, "rsg", es)
    TM = {s: kb.sb([128, 4, 768], BF16, "rTM" + s, es) for s in STRM}
    f = [kb.sb([128, NT], F32, "rf%d" % i, es) for i in range(10)]
    pw = kb.ps([128, NT], F32, "rpw", es)
    pa = kb.ps([128, NT], F32, "rpa", es)
    pg = kb.ps([128, NT], F32, "rpg", es)
    pss = kb.ps([128, NT], F32, "rpss", es)
    pbs = kb.ps([128, NT], F32, "rpbs", es)
    ptr = [kb.ps([128, 4, 128], F32, "rptr", es) for _ in range(2)]
    zv3 = zT.t[0:2560, :].rearrange("(c p) t -> p c t", p=128)
    kb.memset(TM["r"][:, :, :], 0.0)
    for s in STRM:
        kb.dma("sp", View(scr[s], scr[s].t[S:S + 64, :]), TM["r"][0:64, 0, :])
        kb.dma("sp", View(scr[s], scr[s].t[0:1, :]), TM["r"][0:1, 1, :])
    fm = lambda name: scr[name].t.rearrange("(c p) t -> p c t", p=128)
    itr = 0
    for it in range(S // NT):
        t0 = it * NT
        if it == 0:
            kb.memset(Z[:, :, 0:1], 0.0)
            kb.dma("sp", Z[:, :, 1:NT + 1], View(zT, zv3[:, :, 0:NT]))
        else:
            kb.dma("sp", Z[:, :, :], View(zT, zv3[:, :, t0 - 1:t0 + NT]))
        for c in range(20):
            e = "dve"
            kb.op(e, "tensor_tensor", out=ZS[:, c, :], in0=Z[:, c, 0:NT], in1=Z[:, c, 1:NT + 1], op=ALU.subtract)
            kb.op(e, "scalar_tensor_tensor", out=ZS[:, c, :], in0=ZS[:, c, :], scalar=mu[:, l * 20 + c:l * 20 + c + 1],
                  in1=Z[:, c, 1:NT + 1], op0=ALU.mult, op1=ALU.add)
        kb.op("act", "activation", out=lor[0:64, :], in_=ZS[0:64, 18, :], func=AF.Tanh)
        kb.op("act", "copy", out=lor[64:128, :], in_=ZS[64:128, 18, :])
        kb.op("act", "activation", out=sg[:, :], in_=ZS[:, 19, :], func=AF.Sigmoid)
        kb.dma("sp", View(scr["vsT"], fm("vsT")[:, :, t0:t0 + NT]), ZS[:, 12:18, :])
        for c in range(6):
            cs = slice(c * 128, (c + 1) * 128)
            P = lambda j: View(pr, pr.t[:, l, j, c:c + 1])
            mm(kb, pw[:, :], wup[0:64, cs], lor[0:64, :], True, True)
            mm(kb, pa[:, :], wup[64:128, cs], lor[64:128, :], True, True)
            mm(kb, pg[:, :], gup[:, cs], sg[:, :], True, True)
            e1, dec, a, kk, sq, kkn, t1, kp, bb, gsb = f
            kb.op("act", "activation", out=e1[:, :], in_=pw[:, :], func=AF.Exp, scale=-1.0, bias=P(7))
            kb.op("act", "activation", out=e1[:, :], in_=e1[:, :], func=AF.Ln, bias=1.0)
            kb.op("act", "activation", out=e1[:, :], in_=e1[:, :], func=AF.Exp, scale=-1.0, bias=-0.5)
            kb.op("act", "activation", out=dec[:, :], in_=e1[:, :], func=AF.Exp, scale=-1.0)
            kb.op("act", "activation", out=a[:, :], in_=pa[:, :], func=AF.Sigmoid, bias=P(1))
            kb.op("act", "copy", out=gsb[:, :], in_=pg[:, :])
            kb.dma("sp", View(scr["gT"], fm("gT")[:, c, t0:t0 + NT]), gsb[:, :])
            kb.op("dve", "tensor_scalar", out=kk[:, :], in0=ZS[:, 6 + c, :], scalar1=P(2), scalar2=None, op0=ALU.mult)
            kb.op("dve", "tensor_tensor", out=sq[:, :], in0=kk[:, :], in1=kk[:, :], op=ALU.mult)
            mm(kb, pss[:, :], bo[:, :], sq[:, :], True, True)
            rsqrt_eps(kb, sq[:, :], pss[:, :], 1e-24)
            kb.op("dve", "tensor_tensor", out=kkn[:, :], in0=kk[:, :], in1=sq[:, :], op=ALU.mult)
            kb.op("dve", "tensor_scalar", out=t1[:, :], in0=a[:, :], scalar1=-1.0, scalar2=P(3), op0=ALU.add, op1=ALU.mult)
            kb.op("dve", "scalar_tensor_tensor", out=kp[:, :], in0=t1[:, :], scalar=1.0, in1=ZS[:, 6 + c, :],
                  op0=ALU.add, op1=ALU.mult)
            kb.op("dve", "tensor_tensor", out=bb[:, :], in0=kkn[:, :], in1=a[:, :], op=ALU.mult)
            kb.op("dve", "tensor_scalar", out=kkn[:, :], in0=kkn[:, :], scalar1=-1.0, scalar2=None, op0=ALU.mult)
            kb.op("dve", "scalar_tensor_tensor", out=t1[:, :], in0=ZS[:, c, :], scalar=P(4), in1=kp[:, :],
                  op0=ALU.mult, op1=ALU.mult)
            mm(kb, pbs[:, :], bo[:, :], t1[:, :], True, True)
            kb.op("dve", "tensor_tensor", out=t1[:, :], in0=pbs[:, :], in1=ZS[:, 12 + c, :], op=ALU.mult)
            kb.dma("sp", View(scr["bonT"], fm("bonT")[:, c, t0:t0 + NT]), t1[:, :])
            for s, src in (("r", ZS[:, c, :]), ("ash", kkn[:, :]), ("wh", dec[:, :]), ("b", bb[:, :]), ("k", kp[:, :])):
                pt = ptr[itr % 2]
                itr += 1
                for sub in range(4):
                    kb.op("pe", "transpose", out=pt[:, sub, :], in_=View(src.buf, src.ap[:, sub * 128:(sub + 1) * 128]),
                          identity=ident[:, :])
                if s == "wh":
                    kb.op("act", "copy", out=TM["wh"][:, :, cs], in_=pt[:, :, :])
                    kb.op("dve", "tensor_tensor", out=TM["wl"][:, :, cs], in0=pt[:, :, :], in1=TM["wh"][:, :, cs],
                          op=ALU.subtract)
                else:
                    kb.op("act", "copy", out=TM[s][:, :, cs], in_=pt[:, :, :])
        for s in STRM:
            off = 0 if s == "ash" else 1
            kb.dma("sp", View(scr[s], scr[s].t[t0 + off:t0 + off + NT, :].rearrange("(s p) f -> p s f", p=128)), TM[s][:, :, :])
    kb.barrier()
    es.close()


def emit_rwkv_scan(kb, scr, cst):
    es = ExitStack()
    esel = kb.sb([128, 64, 128], BF16, "esel", es)
    kb.dma("pool", esel[:, :, :], View(cst["esel_d"], cst["esel_d"].t.rearrange("p (t q) -> p t q", q=128)))
    NH = 2
    St = [kb.sb([128, 3, 64], F32, "rS", es) for _ in range(NH)]
    T1 = [kb.sb([128, 3, 64], F32, "rT1", es) for _ in range(NH)]
    tmpa = [kb.sb([128, 3, 64], F32, "rtmpa", es) for _ in range(NH)]
    tmpr = [kb.sb([128, 3, 64], F32, "rtmpr", es) for _ in range(NH)]
    T2 = [[kb.sb([128, 3, 64], F32, "rT2", es) for _ in range(NH)] for _ in range(2)]
    Rsb = [kb.sb([128, 6, 64], F32, "rRsb", es) for _ in range(2)]
    SA = [[kb.sb([128, 64, 3], F32, "rSA", es) for _ in range(NH)] for _ in range(2)]
    YR = [[kb.sb([128, 64, 3], F32, "rYR", es) for _ in range(NH)] for _ in range(2)]
    for c in range(NH):
        kb.memset(St[c][:, :, :], 0.0)
        kb.memset(SA[1][c][:, :, :], 0.0)
    tiles = [{s: kb.sb([128, 6, 64], BF16, "rt" + s, es) for s in STRM} for _ in range(2)]
    vt = [kb.sb([128, 6, 64], F32, "rvt", es) for _ in range(2)]
    yb = [kb.sb([128, 6, 64], F32, "ryb", es) for _ in range(2)]
    pR = kb.ps([128, 512], F32, "pR", es)
    pA = kb.ps([128, 512], F32, "pA", es)
    pW = [kb.ps([128, 512], F32, "pW", es) for _ in range(2)]
    pB = [kb.ps([128, 512], F32, "pB", es) for _ in range(2)]
    pK = [kb.ps([128, 512], F32, "pK", es) for _ in range(2)]
    vv = scr["vsT"].t.rearrange("(c p) t -> p c t", p=128)
    yv = scr["yT"].t.rearrange("(c p) t -> p c t", p=128)
    f2 = lambda v: View(v.buf, v.ap.rearrange("p a b -> p (a b)"))
    h3 = lambda v: View(v.buf, v.ap.rearrange("p (a b) -> p a b", b=64))
    sa_prev = SA[1]
    sa_idx = 63
    st = 0
    for n in range(S // 64):
        t0 = n * 64
        tl = tiles[n % 2]
        for s in STRM:
            src = scr[s].t[t0 + 1:t0 + 65, :].rearrange("t (hf hp j) -> hp t hf j", hp=2, j=64)
            for hp in range(2):
                kb.dma("sp", tl[s][hp * 64:(hp + 1) * 64, :, :], View(scr[s], src[hp]))
        kb.dma("sp", vt[n % 2][:, :, :], View(scr["vsT"], vv[:, :, t0:t0 + 64]))
        sa_cur = SA[n % 2]
        yr_cur = YR[n % 2]
        for tp in range(64):
            E = esel[:, tp, :]
            w_, b_, k_ = pW[st % 2], pB[st % 2], pK[st % 2]
            t2 = T2[st % 2]
            rsb = Rsb[st % 2]
            st += 1
            mm(kb, k_[:, 0:384], E, f2(tl["k"][:, :, :]), True, True)
            mm(kb, w_[:, 0:384], E, f2(tl["wh"][:, :, :]), True, False)
            mm(kb, w_[:, 0:384], E, f2(tl["wl"][:, :, :]), False, True)
            mm(kb, b_[:, 0:384], E, f2(tl["b"][:, :, :]), True, True)
            mm(kb, pA[:, 0:384], E, f2(tl["ash"][:, :, :]), True, True)
            mm(kb, pR[:, 0:384], E, f2(tl["r"][:, :, :]), True, True)
            for hf in range(6):
                kb.op("act", "activation", out=t2[hf // 3][:, hf % 3, :], in_=k_[:, hf * 64:(hf + 1) * 64], func=AF.Copy,
                      scale=vt[n % 2][:, hf, tp:tp + 1])
            kb.op("act", "copy", out=f2(rsb[:, :, :]), in_=pR[:, 0:384])
            hs = [slice(c * 192, (c + 1) * 192) for c in range(NH)]
            for c in range(NH):
                kb.op("dve", "tensor_tensor", out=f2(St[c][:, :, :]), in0=f2(St[c][:, :, :]), in1=w_[:, hs[c]], op=ALU.mult)
            for c in range(NH):
                sab = View(sa_prev[c], sa_prev[c].t[:, sa_idx, :].unsqueeze(2).to_broadcast([128, 3, 64]))
                kb.op("dve", "tensor_tensor", out=T1[c][:, :, :], in0=h3(b_[:, hs[c]]), in1=sab, op=ALU.mult)
            for c in range(NH):
                kb.op("dve", "tensor_tensor", out=St[c][:, :, :], in0=St[c][:, :, :], in1=T1[c][:, :, :], op=ALU.add)
            for c in range(NH):
                kb.op("dve", "tensor_tensor", out=St[c][:, :, :], in0=St[c][:, :, :], in1=t2[c][:, :, :], op=ALU.add)
            for c in range(NH):
                kb.op("pool", "tensor_tensor", out=tmpr[c][:, :, :], in0=St[c][:, :, :], in1=rsb[:, 3 * c:3 * c + 3, :], op=ALU.mult)
            for c in range(NH):
                kb.op("dve", "tensor_tensor", out=tmpa[c][:, :, :], in0=h3(pA[:, hs[c]]), in1=St[c][:, :, :], op=ALU.mult)
            for c in range(NH):
                kb.op("dve", "tensor_reduce", out=sa_cur[c][:, tp, :], in_=tmpa[c][:, :, :], axis=AX.X, op=ALU.add)
            for c in range(NH):
                kb.op("dve", "tensor_reduce", out=yr_cur[c][:, tp, :], in_=tmpr[c][:, :, :], axis=AX.X, op=ALU.add)
            sa_prev, sa_idx = sa_cur, tp
        ybt = yb[n % 2]
        for c in range(NH):
            kb.op("act", "copy", out=ybt[:, 3 * c:3 * c + 3, :], in_=View(yr_cur[c], yr_cur[c].t.rearrange("p t c -> p c t")))
        kb.dma("sp", View(scr["yT"], yv[:, :, t0:t0 + 64]), ybt[:, :, :])
    kb.barrier()
    es.close()


def emit_rwkv_post(kb, l, scr, mixT, cst):
    es = ExitStack()
    NT = 512
    bo64 = cst["bo64"]
    pr = cst["rwp"]
    y = kb.sb([128, NT], F32, "py", es)
    g = kb.sb([128, NT], F32, "pgg", es)
    bn = kb.sb([128, NT], F32, "pbn", es)
    sq = kb.sb([128, NT], F32, "psq", es)
    ob = [kb.sb([128, NT], BF16, "pob", es) for _ in range(2)]
    pm = kb.ps([128, NT], F32, "ppm", es)
    pq = kb.ps([128, NT], F32, "ppq", es)
    fm = lambda name: scr[name].t.rearrange("(c p) t -> p c t", p=128)
    i = 0
    for it in range(S // NT):
        t0 = it * NT
        for c in range(6):
            P = lambda j: View(pr, pr.t[:, l, j, c:c + 1])
            kb.dma("sp", y[:, :], View(scr["yT"], fm("yT")[:, c, t0:t0 + NT]))
            kb.dma("sp", g[:, :], View(scr["gT"], fm("gT")[:, c, t0:t0 + NT]))
            kb.dma("sp", bn[:, :], View(scr["bonT"], fm("bonT")[:, c, t0:t0 + NT]))
            mm(kb, pm[:, :], bo64[:, :], y[:, :], True, True)
            kb.op("act", "activation", out=sq[:, :], in_=y[:, :], func=AF.Square)
            mm(kb, pq[:, :], bo64[:, :], sq[:, :], True, True)
            kb.op("dve", "tensor_tensor", out=y[:, :], in0=y[:, :], in1=pm[:, :], op=ALU.subtract)
            kb.op("act", "activation", out=sq[:, :], in_=pm[:, :], func=AF.Square)
            kb.op("dve", "tensor_tensor", out=sq[:, :], in0=pq[:, :], in1=sq[:, :], op=ALU.subtract)
            rsqrt_eps(kb, sq[:, :], sq[:, :], 64e-5)
            kb.op("dve", "tensor_tensor", out=y[:, :], in0=y[:, :], in1=sq[:, :], op=ALU.mult)
            kb.op("act", "activation", out=y[:, :], in_=y[:, :], func=AF.Identity, scale=P(5), bias=P(6))
            kb.op("dve", "tensor_tensor", out=y[:, :], in0=y[:, :], in1=bn[:, :], op=ALU.add)
            o = ob[i % 2]
            i += 1
            kb.op("dve", "tensor_tensor", out=o[:, :], in0=y[:, :], in1=g[:, :], op=ALU.mult)
            kb.dma("sp", View(mixT, mixT.t[c * 128:(c + 1) * 128, t0:t0 + NT]), o[:, :])
    kb.barrier()
    es.close()


def host_consts():
    c = {}
    c["c_onesf"] = np.full((128, 128), 1.0 / 2048.0, np.float32)
    c["c_ones512"] = np.full((128, 128), 1.0 / 512.0, np.float32)
    c["c_ones256"] = np.full((128, 128), 1.0 / 256.0, np.float32)
    c["c_ones1"] = np.full((128, 128), 1.0, np.float32)
    c["c_ident"] = np.eye(128, dtype=np.float32)
    c["c_blockones"] = blockones_const()
    c["c_bo64"] = blockones_const() / 64.0
    rs = np.zeros((32, 32, 128), np.float32)
    for e in range(32):
        rs[e, e, :] = 1.0
    c["c_rowsel"] = rs.reshape(32, 32 * 128)
    c["c_dbias"] = dil_consts()
    c["c_rope"] = rope_consts()
    c["c_mmask"] = mla_masks()
    c["c_esel"] = esel_const()
    return c


CONST_SHAPES = {"c_onesf": [128, 128], "c_ones512": [128, 128], "c_ones256": [128, 128], "c_ones1": [128, 128],
                "c_ident": [128, 128], "c_blockones": [128, 128], "c_bo64": [128, 128], "c_rowsel": [32, 4096],
                "c_dbias": [128, 3072], "c_rope": [64, 2 * S], "c_mmask": [128, 2048], "c_esel": [128, 8192]}


def build(phases="ABCDEFG", nlayers=DEPTH, test=False, ext_mix=False, ext_z=False):
    kb = KB()
    L = nlayers
    ext = lambda n, shp, dt=F32: kb.dram(n, shp, dt, "ExternalInput")
    scr = lambda n, shp, dt=F32: kb.dram(n, shp, dt, "ExternalOutput" if test else "Internal")
    x_in = ext("xT_in", [D, S])
    pT = ext("pT", [L, 256, S])
    w_in = ext("w_in", [L, D, IN_DIM])
    w_out = ext("w_out", [L, D, D])
    lnp = {n: ext(n, [128, L * 16]) for n in ("ln1_g", "ln1_b", "ln2_g", "ln2_b")}
    rw = ext("router_w", [D, 32]); rb = ext("router_b", [32])
    wg = ext("moe_w_gate", [L, 32, D, 512]); wu = ext("moe_w_up", [L, 32, D, 512]); wd = ext("moe_w_down", [L, 32, 512, D])
    wpp = ext("ple_w_proj", [L, 256, D]); wpg = ext("ple_w_gate", [L, D, D])
    rwp_d = ext("rwp", [128, L * 8 * 6]); rwmu_d = ext("rwmu", [128, L * 20])
    qag_d = ext("qag", [128, L * 4]); kvag_d = ext("kvag", [128, L * 2])
    w_up = ext("rwkv_w_up", [L, 64, 768]); a_up = ext("rwkv_a_up", [L, 64, 768]); g_up = ext("rwkv_g_up", [L, 128, 768])
    wuq = ext("mla_w_uq", [L, 512, 1152]); wukv = ext("mla_w_ukv", [L, 256, 1536])
    cd = {n: ext(n, shp) for n, shp in CONST_SHAPES.items()}
    outT = kb.dram("outT", [D, S], F32, "ExternalOutput")
    xT = scr("xT", [D, S])
    zT = ext("zT_in", [ZROWS, S]) if ext_z else scr("zT", [ZROWS, S])
    zv = ext("zv_in", [S, 1536]) if ext_z else scr("zv", [S, 1536])
    mixT = ext("mixT_in", [D, S], BF16) if ext_mix else scr("mixT", [D, S], BF16)
    x1T = scr("x1T", [D, S]); x1b = scr("x1b", [D, S], BF16); gates = scr("gatesT", [32, S]); ffnT = scr("ffnT", [D, S])
    rscr = {s: scr("strm_" + s, [S + 64, 768], BF16) for s in STRM}
    for n in ("vsT", "gT", "bonT", "yT"):
        rscr[n] = scr(n, [768, S])
    mscr = {"qn": scr("qn", [6, 128, S], BF16), "qr": scr("qr", [6, 64, S], BF16)}
    wsc = [[kb.dram("wsc_%d_%d" % (e, j), [128, 16, 512] if j < 2 else [128, 4, 2048], BF16, "Internal") for j in range(3)]
           for e in range(32)]

    es = ExitStack()
    cst = {"rowsel_d": cd["c_rowsel"], "dbias_d": cd["c_dbias"], "rope_d": cd["c_rope"], "mmask_d": cd["c_mmask"],
           "esel_d": cd["c_esel"]}

    def cload(name, src, shape, view=None, dt=F32, q="sp"):
        t = kb.sb(shape, dt, name, es)
        full = tuple(slice(None) for _ in shape)
        kb.dma(q, t[full], view if view is not None else View(src, src.t))
        cst[name] = t
    for n in ("onesf", "ones512", "ones256", "ident", "blockones", "bo64"):
        cload(n, cd["c_" + n], [128, 128])
    cload("onesb", cd["c_ones1"], [128, 128], dt=BF16, q="pool")
    cload("ln1g", lnp["ln1_g"], [128, L * 16]); cload("ln1b", lnp["ln1_b"], [128, L * 16])
    cload("ln2g", lnp["ln2_g"], [128, L * 16]); cload("ln2b", lnp["ln2_b"], [128, L * 16])
    cload("rw", rw, [128, 16, 32], View(rw, rw.t.rearrange("(c p) e -> p c e", p=128)))
    cload("rb", rb, [128, 32], View(rb, rb.t.partition_broadcast(128)))
    cload("rwp", rwp_d, [128, L, 8, 6], View(rwp_d, rwp_d.t.rearrange("p (l j c) -> p l j c", l=L, j=8)))
    cload("rwmu", rwmu_d, [128, L * 20])
    cload("qag", qag_d, [128, L * 4]); cload("kvag", kvag_d, [128, L * 2])
    rwp = cst["rwp"]
    for l in range(L):
        kb.op("dve", "tensor_scalar", out=rwp[:, l, 7, :], in0=rwp[:, l, 0, :], scalar1=-1.0, scalar2=None, op0=ALU.mult)
    xcur = x_in
    for l in range(L):
        xnext = outT if l == L - 1 else xT
        if "A" in phases:
            emit_inproj(kb, xcur, Buf(w_in.t[l], "w_in_l"), zT, zv)
        if "B" in phases:
            wts = {"w_up": Buf(w_up.t[l], "wup_l"), "a_up": Buf(a_up.t[l], "aup_l"), "g_up": Buf(g_up.t[l], "gup_l")}
            emit_rwkv_prep(kb, l, zT, wts, rscr, cst)
            emit_rwkv_scan(kb, rscr, cst)
            emit_rwkv_post(kb, l, rscr, mixT, cst)
        if "C" in phases:
            emit_dil(kb, zT, zv, mixT, cst)
        if "D" in phases:
            emit_mla(kb, l, zT, Buf(wuq.t[l], "wuq_l"), Buf(wukv.t[l], "wukv_l"), mixT, cst, mscr)
        if "E" in phases:
            emit_outproj(kb, l, mixT, Buf(w_out.t[l], "w_out_l"), xcur, x1T, x1b, gates, cst)
        if "F" in phases:
            emit_moe(kb, x1b, gates, Buf(wg.t[l], "wg_l"), Buf(wu.t[l], "wu_l"), Buf(wd.t[l], "wd_l"), ffnT, cst, wsc)
        if "G" in phases:
            emit_ple(kb, l, x1T, ffnT, Buf(pT.t[l], "pT_l"), Buf(wpg.t[l], "wpg_l"), Buf(wpp.t[l], "wpp_l"), xnext, cst)
        xcur = xnext
    es.close()
    return kb.finish()


def fm_cols(a, nch):
    L = a.shape[0]
    return np.ascontiguousarray(a.reshape(L, nch, 128).transpose(2, 0, 1).reshape(128, L * nch))


def host_inputs(inp, b, L=DEPTH):
    f = lambda k: np.asarray(inp[k], np.float32)
    im = dict(host_consts())
    im["xT_in"] = np.ascontiguousarray(f("x")[b].T)
    im["pT"] = np.ascontiguousarray(f("p")[:L, b].transpose(0, 2, 1))
    for k in ("w_in", "w_out", "moe_w_gate", "moe_w_up", "moe_w_down", "ple_w_proj", "ple_w_gate",
              "rwkv_w_up", "rwkv_a_up", "rwkv_g_up", "mla_w_uq", "mla_w_ukv"):
        im[k] = np.ascontiguousarray(f(k)[:L])
    im["router_w"] = f("router_w")
    im["router_b"] = f("router_b")
    for k in ("ln1_g", "ln1_b", "ln2_g", "ln2_b"):
        im[k] = fm_cols(f(k)[:L], 16)
    names = ("rwkv_w0", "rwkv_a0", "rwkv_k_k", "rwkv_k_a", "rwkv_r_k", "rwkv_gn_g", "rwkv_gn_b", "rwkv_w0")
    rwp = np.stack([f(n)[:L].reshape(L, 6, 128) for n in names], 1)
    im["rwp"] = np.ascontiguousarray(rwp.transpose(3, 0, 1, 2).reshape(128, L * 8 * 6))
    im["rwmu"] = fm_cols(f("rwkv_mu")[:L], 20)
    im["qag"] = fm_cols(f("mla_qa_g")[:L], 4)
    im["kvag"] = fm_cols(f("mla_kva_g")[:L], 2)
    return im


_PROG = {}


def kernel(**inputs):
    if "nc" not in _PROG:
        _PROG["nc"] = build()
    in_maps = [host_inputs(inputs, b) for b in range(B)]
    res = run_bass_kernel_spmd(_PROG["nc"], in_maps, core_ids=list(range(NCORES)))
    out = np.stack([np.asarray(res.results[b]["outT"], np.float32).T for b in range(B)], 0)
    return np.ascontiguousarray(out)
```
